# Optimizing a Trainium2 kernel written in Bass

```python
import math
import jax, jax.numpy as jnp
from jax import lax
import numpy as np

D_MODEL = 4096
BATCH = 2
SEQ = 4096
DEPTH = 4

CTX_LEN = 256
GRID_W = 64
Q_BLOCK = 128
HEAD_DIM = 128
A_HEADS = D_MODEL // (4 * HEAD_DIM)
A_QK_DIM = HEAD_DIM // 2
B_HEADS = 3 * D_MODEL // (8 * HEAD_DIM)
B_KV_HEADS = B_HEADS // 3
C_HEADS = 3 * D_MODEL // (8 * HEAD_DIM)
NA_ROWS = 8
NA_COLS = 16
A_WIDTH = A_HEADS * HEAD_DIM
B_WIDTH = B_HEADS * HEAD_DIM
B_KV_WIDTH = B_KV_HEADS * HEAD_DIM
C_WIDTH = C_HEADS * HEAD_DIM
MIX_WIDTH = A_WIDTH + B_WIDTH + C_WIDTH
IN_SPLITS = (A_WIDTH, A_WIDTH, A_WIDTH, B_WIDTH, B_KV_WIDTH, B_KV_WIDTH, C_WIDTH, C_WIDTH, C_WIDTH)
IN_HEADS = (A_HEADS, A_HEADS, A_HEADS, B_HEADS, B_KV_HEADS, B_KV_HEADS, C_HEADS, C_HEADS, C_HEADS)
IN_WIDTH = sum(IN_SPLITS)
N_EXPERTS = 16
EC_CAPACITY = 2
EXPERT_FF = 384
N_MOD = 6
ROPE_BASE = 10000.0
EPS = 1e-6

kernel_name = 'hybrid_diffusion_block'


def rms_norm(x, g):
    xf = x.astype(jnp.float32)
    y = xf * lax.rsqrt(jnp.mean(xf * xf, axis=-1, keepdims=True) + EPS)
    return (y * g.astype(jnp.float32)).astype(x.dtype)


def rope_1d(x, pos):
    half = x.shape[-1] // 2
    freqs = ROPE_BASE ** (-jnp.arange(half, dtype=jnp.float32) / half)
    ang = pos.astype(jnp.float32)[:, None] * freqs
    cos, sin = jnp.cos(ang)[:, None, :], jnp.sin(ang)[:, None, :]
    xf = x.astype(jnp.float32)
    x1, x2 = xf[..., :half], xf[..., half:]
    return jnp.concatenate([x1 * cos - x2 * sin, x2 * cos + x1 * sin], axis=-1).astype(x.dtype)


def axial_rope(x, row, col):
    half = x.shape[-1] // 2
    return jnp.concatenate([rope_1d(x[..., :half], row), rope_1d(x[..., half:], col)], axis=-1)


def rope_halves(x, row, col):
    return jnp.concatenate([axial_rope(x[..., :A_QK_DIM], row, col),
                            axial_rope(x[..., A_QK_DIM:], row, col)], axis=-1)


def modulate(h, shift, scale):
    return h * (1.0 + scale) + shift


def project_heads(h, w_in):
    p = h @ w_in
    offs = np.cumsum(IN_SPLITS)[:-1].tolist()
    parts = jnp.split(p, offs, axis=-1)
    return [pp.reshape(*pp.shape[:-1], nh, HEAD_DIM) for pp, nh in zip(parts, IN_HEADS)]


def sweep_query_blocks(fn, q):
    b, n = q.shape[:2]
    nb = n // Q_BLOCK
    qb = jnp.moveaxis(q.reshape(b, nb, Q_BLOCK, *q.shape[2:]), 1, 0)
    out = lax.map(fn, qb)
    return jnp.moveaxis(out, 0, 1).reshape(b, n, *out.shape[3:])


def diff_attention(q, k, v, lam, subln_g, lam_init):
    scale = A_QK_DIM ** -0.5
    s1 = jnp.einsum('bqhd,bkhd->bhqk', q[..., :A_QK_DIM], k[..., :A_QK_DIM]).astype(jnp.float32) * scale
    s2 = jnp.einsum('bqhd,bkhd->bhqk', q[..., A_QK_DIM:], k[..., A_QK_DIM:]).astype(jnp.float32) * scale
    p = jax.nn.softmax(s1, axis=-1) - lam * jax.nn.softmax(s2, axis=-1)
    o = jnp.einsum('bhqk,bkhd->bqhd', p.astype(v.dtype), v)
    return rms_norm(o, subln_g) * (1.0 - lam_init)


def gqa_attention(q, k, v):
    b, nq, h, d = q.shape
    kvh = k.shape[2]
    qg = q.reshape(b, nq, kvh, h // kvh, d)
    s = jnp.einsum('bqhgd,bkhd->bhgqk', qg, k).astype(jnp.float32) * (d ** -0.5)
    p = jax.nn.softmax(s, axis=-1).astype(v.dtype)
    o = jnp.einsum('bhgqk,bkhd->bqhgd', p, v)
    return o.reshape(b, nq, h, d)


def neighbourhood_attention(q, k, v, k_ctx, v_ctx, rpb, rows):
    b, n, h, d = q.shape
    kh = min(NA_ROWS, rows)
    scale = d ** -0.5
    qg = q.reshape(b, rows, GRID_W, h, d)
    kg = k.reshape(b, rows, GRID_W, h, d)
    vg = v.reshape(b, rows, GRID_W, h, d)
    qcol = jnp.arange(GRID_W)
    col_idx = jnp.clip(qcol - NA_COLS // 2, 0, GRID_W - NA_COLS)[:, None] + jnp.arange(NA_COLS)
    bias_cols = rpb[:, :, col_idx - qcol[:, None] + NA_COLS - 1]

    def one_row(r):
        rs = jnp.clip(r - kh // 2, 0, rows - kh)
        q_r = lax.dynamic_index_in_dim(qg, r, axis=1, keepdims=False)
        k_r = lax.dynamic_slice_in_dim(kg, rs, kh, axis=1)[:, :, col_idx]
        v_r = lax.dynamic_slice_in_dim(vg, rs, kh, axis=1)[:, :, col_idx]
        bias = bias_cols[:, rs + jnp.arange(kh) - r + NA_ROWS - 1]
        s_loc = (jnp.einsum('bqhd,bkqwhd->bhqkw', q_r, k_r).astype(jnp.float32) * scale
                 + jnp.transpose(bias, (0, 2, 1, 3)).astype(jnp.float32)[None])
        s_ctx = jnp.einsum('bqhd,bkhd->bhqk', q_r, k_ctx).astype(jnp.float32) * scale
        n_loc = kh * NA_COLS
        p = jax.nn.softmax(jnp.concatenate([s_loc.reshape(b, h, GRID_W, n_loc), s_ctx], axis=-1), axis=-1)
        p = p.astype(v.dtype)
        p_loc = p[..., :n_loc].reshape(b, h, GRID_W, kh, NA_COLS)
        return (jnp.einsum('bhqkw,bkqwhd->bqhd', p_loc, v_r)
                + jnp.einsum('bhqk,bkhd->bqhd', p[..., n_loc:], v_ctx))

    out = lax.map(one_row, jnp.arange(rows))
    return jnp.moveaxis(out, 0, 1).reshape(b, n, h, d)


def merge_heads(oa, ob, oc, w_out):
    b, n = oa.shape[:2]
    return jnp.concatenate([oa.reshape(b, n, -1), ob.reshape(b, n, -1), oc.reshape(b, n, -1)], axis=-1) @ w_out


def token_mixer(hx, hc, w_in, w_out, a_lambda, a_subln_g, b_q_norm_g, b_k_norm_g, c_rpb,
                lam_init, row, col, rows, need_ctx):
    qa_x, ka_x, va_x, qb_x, kb_x, vb_x, qc_x, kc_x, vc_x = project_heads(hx, w_in)
    qa_c, ka_c, va_c, qb_c, kb_c, vb_c, qc_c, kc_c, vc_c = project_heads(hc, w_in)

    lv = a_lambda.astype(jnp.float32)
    lam = jnp.exp(jnp.sum(lv[0] * lv[1])) - jnp.exp(jnp.sum(lv[2] * lv[3])) + lam_init
    ka_all = jnp.concatenate([ka_c, rope_halves(ka_x, row, col)], axis=1)
    va_all = jnp.concatenate([va_c, va_x], axis=1)
    oa_x = sweep_query_blocks(lambda qblk: diff_attention(qblk, ka_all, va_all, lam, a_subln_g, lam_init),
                              rope_halves(qa_x, row, col))

    kb_c = rms_norm(kb_c, b_k_norm_g)
    kb_all = jnp.concatenate([kb_c, axial_rope(rms_norm(kb_x, b_k_norm_g), row, col)], axis=1)
    vb_all = jnp.concatenate([vb_c, vb_x], axis=1)
    ob_x = sweep_query_blocks(lambda qblk: gqa_attention(qblk, kb_all, vb_all),
                              axial_rope(rms_norm(qb_x, b_q_norm_g), row, col))

    oc_x = neighbourhood_attention(qc_x, kc_x, vc_x, kc_c, vc_c, c_rpb, rows)

    out_x = merge_heads(oa_x, ob_x, oc_x, w_out)
    if not need_ctx:
        return out_x, None
    oa_c = diff_attention(qa_c, ka_c, va_c, lam, a_subln_g, lam_init)
    ob_c = gqa_attention(rms_norm(qb_c, b_q_norm_g), kb_c, vb_c)
    oc_c = gqa_attention(qc_c, kc_c, vc_c)
    return out_x, merge_heads(oa_c, ob_c, oc_c, w_out)


def expert_choice_ffn(h, w_router, w_gate, w_up, w_down):
    b, n, _ = h.shape
    cap = EC_CAPACITY * n // N_EXPERTS
    aff = jax.nn.softmax((h @ w_router).astype(jnp.float32), axis=-1)
    gates, idx = lax.top_k(jnp.swapaxes(aff, 1, 2), cap)
    bidx = jnp.arange(b)[:, None, None]
    xg = h[bidx, idx]
    hid = jax.nn.silu(jnp.einsum('becd,edf->becf', xg, w_gate)) * jnp.einsum('becd,edf->becf', xg, w_up)
    ye = jnp.einsum('becf,efd->becd', hid, w_down) * gates[..., None].astype(h.dtype)
    return jnp.zeros_like(h).at[bidx, idx].add(ye)


def setup_inputs(seed: int = 0) -> dict:
    key = jax.random.key(seed)
    ks = jax.random.split(key, 20)
    D = D_MODEL

    def nrm(k, shape, scale):
        return jax.random.normal(k, shape, jnp.float32) * scale

    return {
        'x': nrm(ks[0], (BATCH, SEQ, D), 1.0),
        'c': nrm(ks[1], (BATCH, D), 1.0),
        'ctx': nrm(ks[2], (BATCH, CTX_LEN, D), 1.0),
        'c_ctx': nrm(ks[3], (D,), 1.0),
        'w_ada': nrm(ks[4], (DEPTH, D, N_MOD * D), 0.5 * D ** -0.5),
        'b_ada': nrm(ks[5], (DEPTH, N_MOD * D), 0.01),
        'norm1_g': 1.0 + nrm(ks[6], (DEPTH, D), 0.01),
        'norm2_g': 1.0 + nrm(ks[7], (DEPTH, D), 0.01),
        'w_in': nrm(ks[8], (DEPTH, D, IN_WIDTH), D ** -0.5),
        'w_out': nrm(ks[9], (DEPTH, MIX_WIDTH, D), MIX_WIDTH ** -0.5),
        'a_lambda': nrm(ks[10], (DEPTH, 4, A_QK_DIM), 0.1),
        'a_subln_g': 1.0 + nrm(ks[11], (DEPTH, HEAD_DIM), 0.01),
        'b_q_norm_g': 1.0 + nrm(ks[12], (DEPTH, HEAD_DIM), 0.01),
        'b_k_norm_g': 1.0 + nrm(ks[13], (DEPTH, HEAD_DIM), 0.01),
        'c_rpb': nrm(ks[14], (DEPTH, C_HEADS, 2 * NA_ROWS - 1, 2 * NA_COLS - 1), 0.1),
        'w_router': nrm(ks[15], (DEPTH, D, N_EXPERTS), D ** -0.5),
        'w_e_gate': nrm(ks[16], (DEPTH, N_EXPERTS, D, EXPERT_FF), D ** -0.5),
        'w_e_up': nrm(ks[17], (DEPTH, N_EXPERTS, D, EXPERT_FF), D ** -0.5),
        'w_e_down': nrm(ks[18], (DEPTH, N_EXPERTS, EXPERT_FF, D), EXPERT_FF ** -0.5),
        'final_g': 1.0 + nrm(ks[19], (D,), 0.01),
    }


def reference(x, c, ctx, c_ctx, w_ada, b_ada, norm1_g, norm2_g, w_in, w_out, a_lambda, a_subln_g,
              b_q_norm_g, b_k_norm_g, c_rpb, w_router, w_e_gate, w_e_up, w_e_down, final_g):
    n = x.shape[1]
    rows = n // GRID_W
    t = jnp.arange(n)
    row, col = t // GRID_W, t % GRID_W
    silu_c = jax.nn.silu(c)
    silu_cc = jax.nn.silu(c_ctx)[None]
    for l in range(DEPTH):
        need_ctx = l < DEPTH - 1
        lam_init = 0.8 - 0.6 * math.exp(-0.3 * l)
        sh1, sc1, g1, sh2, sc2, g2 = jnp.split((silu_c @ w_ada[l] + b_ada[l])[:, None, :], N_MOD, axis=-1)
        csh1, csc1, cg1, csh2, csc2, cg2 = jnp.split((silu_cc @ w_ada[l] + b_ada[l])[:, None, :], N_MOD, axis=-1)
        hx = modulate(rms_norm(x, norm1_g[l]), sh1, sc1)
        hc = modulate(rms_norm(ctx, norm1_g[l]), csh1, csc1)
        ox, oc = token_mixer(hx, hc, w_in[l], w_out[l], a_lambda[l], a_subln_g[l], b_q_norm_g[l],
                             b_k_norm_g[l], c_rpb[l], lam_init, row, col, rows, need_ctx)
        x = x + g1 * ox
        hx = modulate(rms_norm(x, norm2_g[l]), sh2, sc2)
        x = x + g2 * expert_choice_ffn(hx, w_router[l], w_e_gate[l], w_e_up[l], w_e_down[l])
        if need_ctx:
            ctx = ctx + cg1 * oc
            hc = modulate(rms_norm(ctx, norm2_g[l]), csh2, csc2)
            ctx = ctx + cg2 * expert_choice_ffn(hc, w_router[l], w_e_gate[l], w_e_up[l], w_e_down[l])
    return rms_norm(x, final_g)
```

```python
import contextlib
import numpy as np
import concourse.bass as bass
import concourse.mybir as mybir
from concourse.bass_utils import run_bass_kernel_spmd
from concourse.alu_op_type import AluOpType as ALU

F32 = mybir.dt.float32
BF16 = mybir.dt.bfloat16
AF = mybir.ActivationFunctionType
AX = mybir.AxisListType

NCORES = 8
D = 4096
SEQ = 4096
CTX = 256
DEPTH = 4
IN_W = 10240
NEXP = 16
FF = 384
EPS = 1e-6

SAME_ENGINE_SYNC = True
SEM_LIMIT = 30000


class Prog:
    ENGS = ("pe", "dve", "act", "pool", "sp")

    def __init__(self):
        self.nc = bass.Bass("TRN2", target_bir_lowering=False)
        self.stack = contextlib.ExitStack()
        self.ops = {e: [] for e in self.ENGS}
        self.cur = {}
        self.seen = {e: {} for e in self.ENGS}
        self.lastw = {}
        self.readers = {}
        self.semobj = {}
        self.nsem = 0
        self.out_marks = []

    def dram(self, name, shape, dt, kind):
        return self.nc.dram_tensor(name, list(shape), dt, kind=kind).ap()

    def sb(self, name, shape, dt):
        return self.stack.enter_context(self.nc.sbuf_tensor(name, list(shape), dt))

    def ps(self, name, shape, dt=F32):
        return self.stack.enter_context(self.nc.psum_tensor(name, list(shape), dt))

    def _newsem(self, stream):
        s = self.stack.enter_context(self.nc.semaphore(f"s{self.nsem}"))
        sid = self.nsem
        self.nsem += 1
        self.semobj[sid] = s
        self.cur[stream] = [sid, 0]
        return sid

    def _tick(self, stream, inc):
        if stream not in self.cur or self.cur[stream][1] + inc > SEM_LIMIT:
            self._newsem(stream)
        c = self.cur[stream]
        c[1] += inc
        return (c[0], c[1])

    def _deps(self, eng, reads, writes, own_stream):
        need = {}

        def add(sv):
            if sv is None:
                return
            sid, val = sv
            if need.get(sid, 0) < val:
                need[sid] = val

        for k in reads:
            add(self.lastw.get(k))
        for k in writes:
            add(self.lastw.get(k))
            for sid, val in self.readers.get(k, {}).items():
                add((sid, val))
        waits = []
        seen = self.seen[eng]
        for sid, val in need.items():
            if seen.get(sid, 0) >= val:
                continue
            if own_stream is not None and own_stream in self.cur and self.cur[own_stream][0] == sid:
                if eng == "pe" or not SAME_ENGINE_SYNC:
                    continue
            seen[sid] = val
            waits.append((sid, val))
        return waits

    def _mark(self, tick, reads, writes):
        sid, val = tick
        for k in reads:
            r = self.readers.setdefault(k, {})
            if r.get(sid, 0) < val:
                r[sid] = val
        for k in writes:
            self.lastw[k] = (sid, val)
            self.readers[k] = {}

    def op(self, eng, fn, reads=(), writes=()):
        waits = self._deps(eng, reads, writes, eng)
        tick = self._tick(eng, 1)
        self.ops[eng].append((waits, fn, tick, 1))
        self._mark(tick, reads, writes)

    def dma(self, q, out, in_, reads=(), writes=(), stream=None, is_output=False, store=False, **kw):
        if stream is None:
            stream = ("d", reads[0] if (store or not len(writes)) else writes[0])
        waits = self._deps(q, reads, writes, None)
        tick = self._tick(stream, 16)

        def fn(e, out=out, in_=in_, kw=kw):
            return e.dma_start(out=out, in_=in_, **kw)

        self.ops[q].append((waits, fn, tick, 16))
        self._mark(tick, reads, writes)
        if is_output:
            self.out_marks.append(tick)

    def finish(self):
        fin = {}
        for sid, val in self.out_marks:
            fin[sid] = max(fin.get(sid, 0), val)
        for stream, (sid, val) in self.cur.items():
            pass
        final_waits = list(fin.items())
        nc = self.nc
        ops = self.ops
        semobj = self.semobj

        def emit(e, lst, extra=()):
            for waits, fn, (sid, val), inc in lst:
                for ws, wv in waits:
                    e.wait_ge(semobj[ws], wv)
                ins = fn(e)
                ins.then_inc(semobj[sid], inc)
            for ws, wv in extra:
                e.wait_ge(semobj[ws], wv)

        with nc.Block() as block:
            @block.sync
            def _(e):
                emit(e, ops["sp"], final_waits)

            @block.tensor
            def _(e):
                emit(e, ops["pe"])

            @block.vector
            def _(e):
                emit(e, ops["dve"])

            @block.scalar
            def _(e):
                emit(e, ops["act"])

            @block.gpsimd
            def _(e):
                emit(e, ops["pool"], final_waits)
        self.stack.close()
        return nc


def run(prog, in_maps):
    nc = prog.finish()
    res = run_bass_kernel_spmd(nc, in_maps, core_ids=list(range(NCORES)))
    return res.results


ADA_COLS = 6 * D // NCORES


def build_ada():
    p = Prog()
    c3T = p.dram("c3T", [128, 32, 3], F32, "ExternalInput")
    w = p.dram("w", [DEPTH, D, ADA_COLS], F32, "ExternalInput")
    b = p.dram("b", [DEPTH, 3, ADA_COLS], F32, "ExternalInput")
    out = p.dram("out", [DEPTH, 3, ADA_COLS], F32, "ExternalOutput")

    cT = p.sb("cT", [128, 32, 3], F32)
    sT = p.sb("sT", [128, 32, 3], F32)
    bias = p.sb("bias", [3, DEPTH, ADA_COLS], F32)
    res = p.sb("res", [3, DEPTH, ADA_COLS], F32)
    NB = 3
    KG = 8
    wt = [p.sb(f"wt{i}", [128, KG, 512], F32) for i in range(NB)]
    acc = [p.ps(f"acc{i}", [3, 512], F32) for i in range(2)]

    p.dma("sp", cT[:], c3T, writes=["cT"])
    for l in range(DEPTH):
        p.dma("sp", bias[:, l, :], b[l], writes=[("bias", l)])
    p.op("act", lambda e: e.activation(out=sT[:], in_=cT[:], func=AF.Sigmoid), reads=["cT"], writes=["sT"])
    p.op("dve", lambda e: e.tensor_tensor(out=sT[:], in0=sT[:], in1=cT[:], op=ALU.mult), reads=["sT", "cT"], writes=["sT"])

    it = 0
    grp = 0
    for l in range(DEPTH):
        for n in range(ADA_COLS // 512):
            a = acc[grp % 2]
            ak = ("acc", grp % 2)
            for kg in range(32 // KG):
                t = wt[it % NB]
                tk = ("wt", it % NB)
                q = "sp" if it % 2 == 0 else "act"
                src = w[l, kg * KG * 128:(kg + 1) * KG * 128, n * 512:(n + 1) * 512].rearrange("(k p) n -> p k n", p=128)
                p.dma(q, t[:], src, writes=[tk])
                for kk in range(KG):
                    k = kg * KG + kk
                    p.op("pe", lambda e, a=a, t=t, kk=kk, k=k: e.matmul(a[:], lhsT=sT[:, k, :], rhs=t[:, kk, :], start=(k == 0), stop=(k == 31)),
                         reads=["sT", tk], writes=[ak])
                it += 1
            p.op("dve", lambda e, a=a, l=l, n=n: e.tensor_tensor(out=res[:, l, n * 512:(n + 1) * 512], in0=a[:], in1=bias[:, l, n * 512:(n + 1) * 512], op=ALU.add),
                 reads=[ak, ("bias", l)], writes=[("res", l)])
            grp += 1
        p.dma("sp", out[l], res[:, l, :], reads=[("res", l)], is_output=True)
    return p


def run_ada(c, c_ctx, w_ada, b_ada):
    c3 = np.concatenate([c, c_ctx[None]], axis=0).astype(np.float32)
    c3T = np.ascontiguousarray(c3.T.reshape(32, 128, 3).transpose(1, 0, 2))
    in_maps = []
    for i in range(NCORES):
        sl = slice(i * ADA_COLS, (i + 1) * ADA_COLS)
        in_maps.append({
            "c3T": c3T,
            "w": np.ascontiguousarray(w_ada[:, :, sl]),
            "b": np.ascontiguousarray(np.broadcast_to(b_ada[:, None, sl], (DEPTH, 3, ADA_COLS))),
        })
    res = run(build_ada(), in_maps)
    return np.concatenate([r["out"] for r in res], axis=2)


NT = 34
NTOK = NT * 128
NCH = IN_W // 512
ROPE_A = (0, 1, 2, 3)
ROPE_B = (6, 7, 8, 9)
PLAIN_QK = (11, 12, 13, 14, 15, 16)
V_CH = (4, 5, 10, 17, 18, 19)
VSLOT = {4: 0, 5: 512, 10: 1024, 17: 1536, 18: 2048, 19: 2560}
QSLOT = {0: 0, 1: 4, 2: 8, 3: 12, 6: 16, 7: 20, 8: 24, 9: 28, 11: 32, 12: 36, 13: 40, 14: 44, 15: 48, 16: 52}


def emit_norm_T(p, xsrc, t, xt, xn, ss, rstd, ident, tp, hxT, slot, a_vec, b_vec, tag, abk):
    p.dma("sp", xt[:], xsrc[t * 128:(t + 1) * 128, :], writes=["xt"])
    p.op("act", lambda e: e.activation(out=xn[:], in_=xt[:], func=AF.Square, accum_out=ss[:]),
         reads=["xt"], writes=["xn", "ss"])
    p.op("dve", lambda e: e.tensor_scalar(out=rstd[:], in0=ss[:], scalar1=1.0 / D, scalar2=EPS, op0=ALU.mult, op1=ALU.add),
         reads=["ss"], writes=["rstd"])
    p.op("act", lambda e: e.activation(out=rstd[:], in_=rstd[:], func=AF.Sqrt), reads=["rstd"], writes=["rstd"])
    p.op("dve", lambda e: e.reciprocal(out=rstd[:], in_=rstd[:]), reads=["rstd"], writes=["rstd"])
    p.op("act", lambda e: e.activation(out=xn[:], in_=xt[:], func=AF.Copy, scale=rstd[:, 0:1]),
         reads=["xt", "rstd"], writes=["xn"])
    for k4 in range(8):
        tpk = ("tp", k4 % 2)
        tpt = tp[k4 % 2]
        for j in range(4):
            k = k4 * 4 + j
            p.op("pe", lambda e, k=k, j=j, tpt=tpt: e.transpose(out=tpt[:, j, :], in_=xn[:, k * 128:(k + 1) * 128], identity=ident[:]),
                 reads=["xn", "ident"], writes=[tpk])
        for j in range(4):
            k = k4 * 4 + j
            p.op("dve", lambda e, k=k, j=j, tpt=tpt: e.tensor_scalar(
                out=hxT[:, k, slot * 128:(slot + 1) * 128], in0=tpt[:, j, :],
                scalar1=a_vec[:, k:k + 1], scalar2=b_vec[:, k:k + 1], op0=ALU.mult, op1=ALU.add),
                reads=[tpk, abk], writes=[(tag, slot)])


def build_inproj(groups, out_kind="ExternalOutput"):
    p = Prog()
    GMAX = max(len(g) for g in groups)
    xin = p.dram("xin", [NTOK, D], F32, "ExternalInput")
    w_in = p.dram("w_in", [D, IN_W], F32, "ExternalInput")
    modv = p.dram("modv", [128, 5, 32], F32, "ExternalInput")
    identd = p.dram("identd", [128, 128], F32, "ExternalInput")
    ropet = p.dram("ropet", [NT, 128, 4, 128], F32, "ExternalInput")
    bng = p.dram("bng", [128, 2, 128], F32, "ExternalInput")
    qkT = p.dram("qkT", [56, 128, NTOK], BF16, out_kind)
    vtok = p.dram("vtok", [NTOK, 3072], BF16, out_kind)

    ident = p.sb("ident", [128, 128], BF16)
    mv = p.sb("mv", [128, 5, 32], F32)
    ab = p.sb("ab", [128, 4, 32], F32)
    bn = p.sb("bn", [128, 2, 128], F32)
    xt = p.sb("xt", [128, D], F32)
    xn = p.sb("xn", [128, D], BF16)
    ss = p.sb("ss", [128, 1], F32)
    rstd = p.sb("rstd", [128, 1], F32)
    hxT = p.sb("hxT", [128, 32, GMAX * 128], BF16)
    wb = [p.sb(f"wb{i}", [128, 32, 512], BF16) for i in range(2)]
    rt = [p.sb(f"rt{i}", [128, 4, 128], F32) for i in range(2)]
    t1 = p.sb("t1", [128, 512], F32)
    t2 = p.sb("t2", [128, 512], F32)
    nrm = p.sb("nrm", [128, 512], F32)
    raw = p.sb("raw", [128, 512], F32)
    ss4 = p.sb("ss4", [128, 4], F32)
    pp = [p.sb(f"pp{i}", [128, 512], BF16) for i in range(2)]
    ppT = [p.sb(f"ppT{i}", [128, 4, 128], BF16) for i in range(2)]
    tp = [p.ps(f"tp{i}", [128, 4, 128], BF16) for i in range(2)]
    acc = [p.ps(f"acc{i}", [128, 512], F32) for i in range(2)]

    p.dma("pool", ident[:], identd, writes=["ident"])
    p.dma("sp", mv[:], modv, writes=["mv"])
    p.dma("sp", bn[:], bng, writes=["bn"])
    for i, (sc, sh) in enumerate(((1, 2), (3, 4))):
        p.op("dve", lambda e, i=i, sc=sc: e.scalar_tensor_tensor(out=ab[:, 2 * i, :], in0=mv[:, sc, :], scalar=1.0, in1=mv[:, 0, :], op0=ALU.add, op1=ALU.mult),
             reads=["mv"], writes=["lab" if i == 0 else "cab"])
        p.op("dve", lambda e, i=i, sh=sh: e.tensor_copy(out=ab[:, 2 * i + 1, :], in_=mv[:, sh, :]),
             reads=["mv"], writes=["lab" if i == 0 else "cab"])

    ppi = 0
    wi = 0
    for g in groups:
        for s, t in enumerate(g):
            isctx = t < 2
            emit_norm_T(p, xin, t, xt, xn, ss, rstd, ident, tp, hxT, s,
                        ab[:, 2, :] if isctx else ab[:, 0, :], ab[:, 3, :] if isctx else ab[:, 1, :],
                        "hx", "cab" if isctx else "lab")
        for c in range(NCH):
            w = wb[wi % 2]
            wk = ("wb", wi % 2)
            wi += 1
            for kq in range(4):
                p.dma("pool", w[:, kq * 8:(kq + 1) * 8, :],
                      w_in[kq * 1024:(kq + 1) * 1024, c * 512:(c + 1) * 512].rearrange("(k p) n -> p k n", p=128),
                      writes=[wk])
            for s, t in enumerate(g):
                a = acc[(s) % 2]
                ak = ("acc", s % 2)
                for k in range(32):
                    p.op("pe", lambda e, a=a, w=w, k=k, s=s: e.matmul(a[:], lhsT=hxT[:, k, s * 128:(s + 1) * 128], rhs=w[:, k, :], start=(k == 0), stop=(k == 31)),
                         reads=[("hx", s), wk], writes=[ak])
                o = pp[ppi % 2]
                ok = ("pp", ppi % 2)
                oT = ppT[ppi % 2]
                oTk = ("ppT", ppi % 2)
                ppi += 1
                if c in V_CH or c in PLAIN_QK:
                    p.op("act", lambda e, o=o, a=a: e.activation(out=o[:], in_=a[:], func=AF.Copy), reads=[ak], writes=[ok])
                else:
                    isB = c in ROPE_B
                    r = rt[ppi % 2]
                    rk = ("rt", ppi % 2)
                    p.dma("sp", r[:], ropet[t], writes=[rk])
                    src = a
                    srck = ak
                    if isB:
                        gi = 0 if c < 9 else 1
                        for h in range(4):
                            p.op("act", lambda e, h=h, a=a: e.activation(
                                out=t1[:, h * 128:(h + 1) * 128], in_=a[:, h * 128:(h + 1) * 128], func=AF.Square, accum_out=ss4[:, h:h + 1]),
                                reads=[ak], writes=["t1", "ss4"])
                        p.op("dve", lambda e: e.tensor_scalar(out=ss4[:], in0=ss4[:], scalar1=1.0 / 128, scalar2=EPS, op0=ALU.mult, op1=ALU.add),
                             reads=["ss4"], writes=["ss4"])
                        p.op("act", lambda e: e.activation(out=ss4[:], in_=ss4[:], func=AF.Sqrt), reads=["ss4"], writes=["ss4"])
                        p.op("dve", lambda e: e.reciprocal(out=ss4[:], in_=ss4[:]), reads=["ss4"], writes=["ss4"])
                        for h in range(4):
                            p.op("dve", lambda e, h=h, gi=gi, a=a: e.scalar_tensor_tensor(
                                out=nrm[:, h * 128:(h + 1) * 128], in0=a[:, h * 128:(h + 1) * 128], scalar=ss4[:, h:h + 1],
                                in1=bn[:, gi, :], op0=ALU.mult, op1=ALU.mult),
                                reads=[ak, "ss4", "bn"], writes=["nrm"])
                        src = nrm
                        srck = "nrm"
                    ci, si = (2, 3) if isB else (0, 1)
                    hw = 32 if isB else 16
                    nb = 128 // (2 * hw)
                    Cb = r[:, ci, :].unsqueeze(1).broadcast_to([128, 4, 128])
                    sv = src[:].rearrange("p (h b two w) -> p h b two w", h=4, b=nb, two=2, w=hw)
                    Sv = r[:, si, :].rearrange("p (b two w) -> p b two w", b=nb, two=2, w=hw)
                    t2v = t2[:].rearrange("p (h b two w) -> p h b two w", h=4, b=nb, two=2, w=hw)
                    p.op("dve", lambda e, src=src, Cb=Cb: e.tensor_tensor(out=t1[:].rearrange("p (h d) -> p h d", h=4), in0=src[:].rearrange("p (h d) -> p h d", h=4), in1=Cb, op=ALU.mult),
                         reads=[srck, rk], writes=["t1"])
                    for h in range(4):
                        for half in range(2):
                            p.op("dve", lambda e, h=h, half=half, sv=sv, Sv=Sv, t2v=t2v: e.tensor_tensor(
                                out=t2v[:, h, :, half, :], in0=sv[:, h, :, 1 - half, :], in1=Sv[:, :, half, :], op=ALU.mult),
                                reads=[srck, rk], writes=["t2"])
                    p.op("dve", lambda e, o=o: e.tensor_tensor(out=o[:], in0=t1[:], in1=t2[:], op=ALU.add),
                         reads=["t1", "t2"], writes=[ok])
                if c in V_CH:
                    p.dma("sp", vtok[t * 128:(t + 1) * 128, VSLOT[c]:VSLOT[c] + 512], o[:], reads=[ok], writes=[("vtok", t, c)],
                          is_output=(out_kind == "ExternalOutput"), store=True)
                else:
                    tpt = tp[ppi % 2]
                    tpk = ("tp", ppi % 2)
                    for h in range(4):
                        p.op("pe", lambda e, h=h, o=o, tpt=tpt: e.transpose(out=tpt[:, h, :], in_=o[:, h * 128:(h + 1) * 128], identity=ident[:]),
                             reads=[ok, "ident"], writes=[tpk])
                    p.op("act", lambda e, oT=oT, tpt=tpt: e.activation(out=oT[:], in_=tpt[:], func=AF.Copy), reads=[tpk], writes=[oTk])
                    p.dma("sp", qkT[QSLOT[c]:QSLOT[c] + 4, :, t * 128:(t + 1) * 128].rearrange("h d t -> d h t"), oT[:], reads=[oTk],
                          writes=[("qkT", t, c)], is_output=(out_kind == "ExternalOutput"), store=True)
    return p


def rope_tables():
    n = np.arange(SEQ)
    row = (n // 64).astype(np.float64)
    col = (n % 64).astype(np.float64)
    out = np.zeros((NT, 128, 4, 128), np.float32)
    out[0:2, :, 0, :] = 1.0
    out[0:2, :, 2, :] = 1.0

    def blk(pos, half):
        fr = 10000.0 ** (-np.arange(half, dtype=np.float32) / half)
        ang = pos.astype(np.float32)[:, None] * fr[None, :]
        c, s_ = np.cos(ang), np.sin(ang)
        return np.concatenate([c, c], 1), np.concatenate([-s_, s_], 1)

    cr, sr = blk(row, 16)
    cc, sc = blk(col, 16)
    CA = np.concatenate([cr, cc, cr, cc], 1)
    SA = np.concatenate([sr, sc, sr, sc], 1)
    cr, sr = blk(row, 32)
    cc, sc = blk(col, 32)
    CB = np.concatenate([cr, cc], 1)
    SB = np.concatenate([sr, sc], 1)
    lat = np.stack([CA, SA, CB, SB], 1).reshape(32, 128, 4, 128)
    out[2:] = lat
    return out


def fm(v):
    return np.ascontiguousarray(np.asarray(v, np.float32).reshape(32, 128).T)


NEG = -30000.0


def build_attn(heads=None, qblocks=None, crows=None, in_kind="ExternalInput", out_kind="ExternalOutput"):
    p = Prog()
    qkT = p.dram("qkT", [56, 128, NTOK], BF16, in_kind)
    vtok = p.dram("vtok", [NTOK, 3072], BF16, in_kind)
    alam = p.dram("alam", [128, 4, 64], F32, "ExternalInput")
    lconst = p.dram("lconst", [128, 2], F32, "ExternalInput")
    sublg = p.dram("sublg", [128, 128], F32, "ExternalInput")
    cb2d = p.dram("cb2d", [12, 128, 14, 64], F32, "ExternalInput")
    otok = p.dram("otok", [NTOK, D], BF16, out_kind)

    KT = p.sb("KT", [128, NTOK], BF16)
    QT = p.sb("QT", [128, NTOK], BF16)
    V1 = p.sb("V1", [128, NT, 129], BF16)
    V1o = p.sb("V1o", [128, 31, 129], BF16)
    cb2 = p.sb("cb2", [128, 14, 64], F32)
    al = p.sb("al", [128, 4, 64], F32)
    lc = p.sb("lc", [128, 2], F32)
    sg = p.sb("sg", [128, 128], F32)
    lam = p.sb("lam", [128, 4], F32)
    junk = p.sb("junk", [128, 128], F32)
    Pt = [p.sb(f"Pt{i}", [128, 512], BF16) for i in range(2)]
    sc = [p.sb(f"sc{i}", [128, 64], F32) for i in range(2)]
    o1 = p.sb("o1", [128, 4, 128], F32)
    o2 = p.sb("o2", [128, 4, 128], F32)
    rz = p.sb("rz", [128, 4], F32)
    ssq = p.sb("ssq", [128, 4], F32)
    ost = [p.sb(f"ost{i}", [128, 4, 128], BF16) for i in range(2)]
    pss = [p.ps(f"pss{i}", [128, 512], F32) for i in range(2)]
    acc = [p.ps(f"pacc{i}", [128, 512], F32) for i in range(4)]

    p.dma("sp", al[:], alam, writes=["al"])
    p.dma("sp", lc[:], lconst, writes=["lc"])
    p.dma("sp", sg[:], sublg, writes=["sg"])
    p.op("dve", lambda e: e.memset(V1[:, :, 128:129], 1.0), writes=["V1ones"])
    p.op("dve", lambda e: e.memset(V1o[:, :, 128:129], 1.0), writes=["V1oones"])
    for i in range(2):
        p.op("dve", lambda e, i=i: e.tensor_tensor(out=junk[:, 0:64], in0=al[:, 2 * i, :], in1=al[:, 2 * i + 1, :], op=ALU.mult),
             reads=["al"], writes=["junk"])
        p.op("dve", lambda e, i=i: e.reduce_sum(out=lam[:, i:i + 1], in_=junk[:, 0:64], axis=AX.X), reads=["junk"], writes=["lam"])
    p.op("act", lambda e: e.activation(out=lam[:, 0:2], in_=lam[:, 0:2], func=AF.Exp), reads=["lam"], writes=["lam"])
    p.op("dve", lambda e: e.tensor_tensor(out=lam[:, 2:3], in0=lam[:, 0:1], in1=lam[:, 1:2], op=ALU.subtract), reads=["lam"], writes=["lam"])
    p.op("dve", lambda e: e.tensor_tensor(out=lam[:, 2:3], in0=lam[:, 2:3], in1=lc[:, 0:1], op=ALU.add), reads=["lam", "lc"], writes=["lam"])
    p.op("dve", lambda e: e.tensor_scalar(out=lam[:, 3:4], in0=lam[:, 2:3], scalar1=-1.0, scalar2=None, op0=ALU.mult), reads=["lam"], writes=["lam"])
    p.op("dve", lambda e: e.tensor_scalar(out=sg[:], in0=sg[:], scalar1=lc[:, 1:2], scalar2=None, op0=ALU.mult), reads=["sg", "lc"], writes=["sg"])

    cnt = {"s": 0, "o": 0}

    def load_head(qh, kh, vcol, need_odd):
        p.dma("sp", QT[:], qkT[qh], writes=["QT"])
        p.dma("sp", KT[:], qkT[kh], writes=["KT"])
        p.dma("sp", V1[:, :, 0:128], vtok[:, vcol:vcol + 128].rearrange("(j p) d -> p j d", p=128), writes=["V1"])
        if need_odd:
            p.dma("sp", V1o[:, :, 0:128], vtok[320:320 + 31 * 128, vcol:vcol + 128].rearrange("(j p) d -> p j d", p=128), writes=["V1o"])

    def dense_pass(kr, q0, nq, ktiles, scale, fin):
        nqt = nq // 128
        for idx, j in enumerate(ktiles):
            s_ = pss[cnt["s"] % 2]
            sk = ("pss", cnt["s"] % 2)
            pt = Pt[cnt["s"] % 2]
            pk = ("Pt", cnt["s"] % 2)
            cnt["s"] += 1
            p.op("pe", lambda e, s_=s_, j=j: e.matmul(s_[:, 0:nq], lhsT=KT[kr[0]:kr[1], j * 128:(j + 1) * 128], rhs=QT[kr[0]:kr[1], q0:q0 + nq], start=True, stop=True),
                 reads=["KT", "QT"], writes=[sk])
            p.op("act", lambda e, s_=s_, pt=pt: e.activation(out=pt[:, 0:nq], in_=s_[:, 0:nq], func=AF.Exp, scale=scale), reads=[sk], writes=[pk])
            for qi in range(nqt):
                p.op("pe", lambda e, qi=qi, pt=pt, j=j, idx=idx: e.matmul(acc[qi][:, 0:129], lhsT=pt[:, qi * 128:(qi + 1) * 128], rhs=V1[:, j, :],
                                                                   start=(idx == 0), stop=(idx == len(ktiles) - 1)),
                     reads=[pk, "V1", "V1ones"], writes=[("acc", qi)])
        for qi in range(nqt):
            fin(qi)

    def fin_to(dst, dk):
        def f(qi):
            p.op("dve", lambda e, qi=qi: e.reciprocal(out=rz[:, qi:qi + 1], in_=acc[qi][:, 128:129]), reads=[("acc", qi)], writes=[("rz", qi)])
            p.op("act", lambda e, qi=qi: e.activation(out=dst[:, qi, :], in_=acc[qi][:, 0:128], func=AF.Copy, scale=rz[:, qi:qi + 1]),
                 reads=[("acc", qi), ("rz", qi)], writes=[(dk, qi)])
        return f

    def store(o, ok_, q0, nqt, col):
        p.dma("sp", otok[q0:q0 + nqt * 128, col:col + 128].rearrange("(j p) d -> p j d", p=128), o[:, 0:nqt, :],
              reads=[(ok_, qi) for qi in range(nqt)], writes=[("otok", q0, col)], is_output=(out_kind == "ExternalOutput"), store=True,
              stream=("d", ok_))

    blocks = [(0, 256, [0, 1])] + [(256 + 512 * i, 512, list(range(NT))) for i in range(8)]
    if qblocks is not None:
        blocks = [blocks[i] for i in qblocks]
    allheads = [("A", h) for h in range(8)] + [("B", h) for h in range(12)] + [("C", h) for h in range(12)]
    if heads is not None:
        allheads = heads
    for grp, h in allheads:
        if grp == "A":
            load_head(h, 8 + h, 128 * h, False)
            col = 128 * h
            for (q0, nq, kts) in blocks:
                nqt = nq // 128
                o = ost[cnt["o"] % 2]
                ok_ = ("ost", cnt["o"] % 2)
                cnt["o"] += 1
                dense_pass((0, 64), q0, nq, kts, 0.125, fin_to(o1, "o1"))
                dense_pass((64, 128), q0, nq, kts, 0.125, fin_to(o2, "o2"))
                for qi in range(nqt):
                    p.op("dve", lambda e, qi=qi: e.scalar_tensor_tensor(out=o1[:, qi, :], in0=o2[:, qi, :], scalar=lam[:, 3:4], in1=o1[:, qi, :], op0=ALU.mult, op1=ALU.add),
                         reads=[("o1", qi), ("o2", qi), "lam"], writes=[("o1", qi)])
                    p.op("act", lambda e, qi=qi: e.activation(out=junk[:], in_=o1[:, qi, :], func=AF.Square, accum_out=ssq[:, qi:qi + 1]),
                         reads=[("o1", qi)], writes=["junk", ("ssq", qi)])
                    p.op("dve", lambda e, qi=qi: e.tensor_scalar(out=ssq[:, qi:qi + 1], in0=ssq[:, qi:qi + 1], scalar1=1.0 / 128, scalar2=EPS, op0=ALU.mult, op1=ALU.add),
                         reads=[("ssq", qi)], writes=[("ssq", qi)])
                    p.op("act", lambda e, qi=qi: e.activation(out=ssq[:, qi:qi + 1], in_=ssq[:, qi:qi + 1], func=AF.Sqrt), reads=[("ssq", qi)], writes=[("ssq", qi)])
                    p.op("dve", lambda e, qi=qi: e.reciprocal(out=ssq[:, qi:qi + 1], in_=ssq[:, qi:qi + 1]), reads=[("ssq", qi)], writes=[("ssq", qi)])
                    p.op("dve", lambda e, qi=qi, o=o: e.scalar_tensor_tensor(out=o[:, qi, :], in0=o1[:, qi, :], scalar=ssq[:, qi:qi + 1], in1=sg[:], op0=ALU.mult, op1=ALU.mult),
                         reads=[("o1", qi), ("ssq", qi), "sg"], writes=[(ok_, qi)])
                store(o, ok_, q0, nqt, col)
        elif grp == "B":
            load_head(16 + h, 28 + h // 3, 1024 + 128 * (h // 3), False)
            col = 1024 + 128 * h
            sc_ = 128 ** -0.5
            for (q0, nq, kts) in blocks:
                nqt = nq // 128
                o = ost[cnt["o"] % 2]
                ok_ = ("ost", cnt["o"] % 2)
                cnt["o"] += 1
                dense_pass((0, 128), q0, nq, kts, sc_, fin_to(o, ok_))
                store(o, ok_, q0, nqt, col)
        else:
            load_head(32 + h, 44 + h, 1536 + 128 * h, True)
            p.dma("sp", cb2[:], cb2d[h], writes=["cb2"])
            col = 2560 + 128 * h
            sc_ = 128 ** -0.5
            if qblocks is None or 0 in qblocks:
                o = ost[cnt["o"] % 2]
                ok_ = ("ost", cnt["o"] % 2)
                cnt["o"] += 1
                dense_pass((0, 128), 0, 256, [0, 1], sc_, fin_to(o, ok_))
                store(o, ok_, 0, 2, col)
            rows = range(64) if crows is None else crows
            for r in rows:
                rs = min(max(r - 4, 0), 56)
                q0 = 256 + 64 * r
                half = r % 2
                if half == 0 or r == rows[0]:
                    o = ost[cnt["o"] % 2]
                    ok_ = ("ost", cnt["o"] % 2)
                    cnt["o"] += 1
                a = acc[r % 4]
                akey = ("acc", r % 4)
                kts = [("c", 0), ("c", 1)] + [("l", i) for i in range(4)]
                for idx, (kind, i) in enumerate(kts):
                    s_ = pss[cnt["s"] % 2]
                    sk = ("pss", cnt["s"] % 2)
                    pt = Pt[cnt["s"] % 2]
                    pk = ("Pt", cnt["s"] % 2)
                    scb = sc[cnt["s"] % 2]
                    sck = ("sc", cnt["s"] % 2)
                    cnt["s"] += 1
                    if kind == "c":
                        k0 = i * 128
                        vt = V1[:, i, :]
                        vk = "V1"
                    else:
                        row0 = rs + 2 * i
                        k0 = 256 + 64 * row0
                        if row0 % 2 == 0:
                            vt = V1[:, 2 + row0 // 2, :]
                            vk = "V1"
                        else:
                            vt = V1o[:, (row0 - 1) // 2, :]
                            vk = "V1o"
                    p.op("pe", lambda e, s_=s_, k0=k0, q0=q0: e.matmul(s_[:, 0:64], lhsT=KT[:, k0:k0 + 128], rhs=QT[:, q0:q0 + 64], start=True, stop=True),
                         reads=["KT", "QT"], writes=[sk])
                    if kind == "c":
                        p.op("act", lambda e, s_=s_, pt=pt: e.activation(out=pt[:, 0:64], in_=s_[:, 0:64], func=AF.Exp, scale=sc_), reads=[sk], writes=[pk])
                    else:
                        di = rs + 2 * i - r + 7
                        p.op("dve", lambda e, s_=s_, scb=scb, di=di: e.scalar_tensor_tensor(out=scb[:], in0=s_[:, 0:64], scalar=sc_, in1=cb2[:, di, :], op0=ALU.mult, op1=ALU.add),
                             reads=[sk, "cb2"], writes=[sck])
                        p.op("act", lambda e, scb=scb, pt=pt: e.activation(out=pt[:, 0:64], in_=scb[:], func=AF.Exp), reads=[sck], writes=[pk])
                    p.op("pe", lambda e, a=a, pt=pt, vt=vt, idx=idx: e.matmul(a[0:64, 0:129], lhsT=pt[:, 0:64], rhs=vt, start=(idx == 0), stop=(idx == 5)),
                         reads=[pk, vk, vk + "ones"], writes=[akey])
                p.op("dve", lambda e, a=a, r=r: e.reciprocal(out=rz[0:64, r % 4:r % 4 + 1], in_=a[0:64, 128:129]), reads=[akey], writes=[("rz", r % 4)])
                p.op("act", lambda e, a=a, r=r, o=o, half=half: e.activation(out=o[0:64, half * 2, :], in_=a[0:64, 0:128], func=AF.Copy, scale=rz[0:64, r % 4:r % 4 + 1]),
                     reads=[akey, ("rz", r % 4)], writes=[(ok_, half)])
                p.dma("sp", otok[q0:q0 + 64, col:col + 128], o[0:64, half * 2, :], reads=[(ok_, half)], writes=[("otok", q0, col)],
                      is_output=(out_kind == "ExternalOutput"), store=True, stream=("d", ok_, half))
    return p


def build_cb2(rpb):
    qc = np.arange(64)
    cs = np.clip(qc - 8, 0, 48)
    kc = np.arange(64)
    valid = (kc[:, None] >= cs[None, :]) & (kc[:, None] < cs[None, :] + 16)
    dc = np.clip(kc[:, None] - qc[None, :] + 15, 0, 30)
    out = np.full((12, 128, 14, 64), NEG, np.float32)
    for di in range(14):
        for kr in range(2):
            v = rpb[:, di + kr][:, dc]
            out[:, kr * 64:(kr + 1) * 64, di, :] = np.where(valid[None], v, NEG)
    return out


GROUPS4 = [[0, 1]] + [list(range(2 + 4 * i, 6 + 4 * i)) for i in range(8)]
GROUPS9 = [list(range(0, 9)), list(range(9, 18)), list(range(18, 26)), list(range(26, 34))]


def build_outproj(groups=GROUPS9):
    p = Prog()
    otok = p.dram("otok", [NTOK, D], BF16, "ExternalInput")
    xin = p.dram("xin", [NTOK, D], F32, "ExternalInput")
    w_out = p.dram("w_out", [D, D], F32, "ExternalInput")
    gbc = p.dram("gbc", [2, 128, D], F32, "ExternalInput")
    identd = p.dram("identd", [128, 128], F32, "ExternalInput")
    xmid = p.dram("xmid", [NTOK, D], F32, "ExternalOutput")
    GMAX = max(len(g) for g in groups)
    ident = p.sb("ident", [128, 128], BF16)
    ot = p.sb("ot", [128, D], BF16)
    OT = p.sb("OT", [128, 32, GMAX * 128], BF16)
    wb = [p.sb(f"wb{i}", [128, 32, 512], BF16) for i in range(2)]
    gt = p.sb("gt", [128, 2, 512], F32)
    xc = [p.sb(f"xc{i}", [128, 512], F32) for i in range(2)]
    ob = [p.sb(f"ob{i}", [128, 512], F32) for i in range(2)]
    tp = [p.ps(f"tp{i}", [128, 4, 128], BF16) for i in range(2)]
    acc = [p.ps(f"acc{i}", [128, 512], F32) for i in range(2)]
    p.dma("pool", ident[:], identd, writes=["ident"])
    wi = 0
    oi = 0
    for g in groups:
        for s, t in enumerate(g):
            p.dma("sp", ot[:], otok[t * 128:(t + 1) * 128, :], writes=["ot"])
            for k4 in range(8):
                tpt = tp[k4 % 2]
                tpk = ("tp", k4 % 2)
                for j in range(4):
                    k = k4 * 4 + j
                    p.op("pe", lambda e, k=k, j=j, tpt=tpt: e.transpose(out=tpt[:, j, :], in_=ot[:, k * 128:(k + 1) * 128], identity=ident[:]),
                         reads=["ot", "ident"], writes=[tpk])
                p.op("act", lambda e, k4=k4, s=s, tpt=tpt: e.activation(out=OT[:, k4 * 4:(k4 + 1) * 4, s * 128:(s + 1) * 128], in_=tpt[:], func=AF.Copy),
                     reads=[tpk], writes=[("OT", s)])
        for n in range(8):
            w = wb[wi % 2]
            wk = ("wb", wi % 2)
            wi += 1
            for kq in range(4):
                p.dma("pool", w[:, kq * 8:(kq + 1) * 8, :],
                      w_out[kq * 1024:(kq + 1) * 1024, n * 512:(n + 1) * 512].rearrange("(k p) n -> p k n", p=128), writes=[wk])
            p.dma("sp", gt[:], gbc[:, :, n * 512:(n + 1) * 512].rearrange("a p n -> p a n"), writes=["gt"])
            for s, t in enumerate(g):
                a = acc[oi % 2]
                ak = ("acc", oi % 2)
                x_ = xc[oi % 2]
                xk = ("xc", oi % 2)
                o_ = ob[oi % 2]
                ok_ = ("ob", oi % 2)
                oi += 1
                gi = 1 if t < 2 else 0
                p.dma("sp", x_[:], xin[t * 128:(t + 1) * 128, n * 512:(n + 1) * 512], writes=[xk])
                for k in range(32):
                    p.op("pe", lambda e, a=a, w=w, k=k, s=s: e.matmul(a[:], lhsT=OT[:, k, s * 128:(s + 1) * 128], rhs=w[:, k, :], start=(k == 0), stop=(k == 31)),
                         reads=[("OT", s), wk], writes=[ak])
                p.op("dve", lambda e, a=a, o_=o_, gi=gi: e.tensor_tensor(out=o_[:], in0=a[:], in1=gt[:, gi, :], op=ALU.mult), reads=[ak, "gt"], writes=[ok_])
                p.op("dve", lambda e, o_=o_, x_=x_: e.tensor_tensor(out=o_[:], in0=o_[:], in1=x_[:], op=ALU.add), reads=[ok_, xk], writes=[ok_])
                p.dma("sp", xmid[t * 128:(t + 1) * 128, n * 512:(n + 1) * 512], o_[:], reads=[ok_], writes=[("xmid", t, n)], is_output=True, store=True)
    return p


def build_router(niter=30):
    p = Prog()
    xmid = p.dram("xmid", [NTOK, D], F32, "ExternalInput")
    modv = p.dram("modv", [128, 5, 32], F32, "ExternalInput")
    identd = p.dram("identd", [128, 128], F32, "ExternalInput")
    w_r = p.dram("w_r", [D, NEXP], F32, "ExternalInput")
    hx2T = p.dram("hx2T", [32, 128, NTOK], BF16, "ExternalOutput")
    gateT = p.dram("gateT", [NEXP, NTOK], F32, "ExternalOutput")

    ident = p.sb("ident", [128, 128], BF16)
    identf = p.sb("identf", [128, 128], F32)
    mv = p.sb("mv", [128, 5, 32], F32)
    ab = p.sb("ab", [128, 4, 32], F32)
    wr = p.sb("wr", [128, 32, NEXP], BF16)
    xt = p.sb("xt", [128, D], F32)
    xn = p.sb("xn", [128, D], BF16)
    ss = p.sb("ss", [128, 1], F32)
    rstd = p.sb("rstd", [128, 1], F32)
    hT = [p.sb(f"hT{i}", [128, 32, 128], BF16) for i in range(2)]
    aff = p.sb("aff", [128, NEXP], F32)
    mx = p.sb("mx", [128, 2], F32)
    affT = p.sb("affT", [NEXP, NTOK], F32)
    gT = p.sb("gT", [NEXP, NTOK], F32)
    st = p.sb("st", [NEXP, 8], F32)
    tp = [p.ps(f"tp{i}", [128, 4, 128], BF16) for i in range(2)]
    lg = p.ps("lg", [128, NEXP], F32)
    tpf = p.ps("tpf", [NEXP, 128], F32)

    p.dma("pool", ident[:], identd, writes=["ident"])
    p.dma("sp", identf[:], identd, writes=["identf"])
    p.dma("sp", mv[:], modv, writes=["mv"])
    p.dma("pool", wr[:], w_r.rearrange("(k p) n -> p k n", p=128), writes=["wr"])
    for i, (sc, sh) in enumerate(((1, 2), (3, 4))):
        p.op("dve", lambda e, i=i, sc=sc: e.scalar_tensor_tensor(out=ab[:, 2 * i, :], in0=mv[:, sc, :], scalar=1.0, in1=mv[:, 0, :], op0=ALU.add, op1=ALU.mult),
             reads=["mv"], writes=["lab" if i == 0 else "cab"])
        p.op("dve", lambda e, i=i, sh=sh: e.tensor_copy(out=ab[:, 2 * i + 1, :], in_=mv[:, sh, :]), reads=["mv"], writes=["lab" if i == 0 else "cab"])

    for t in range(NT):
        isctx = t < 2
        h = hT[t % 2]
        tag = "hT%d" % (t % 2)
        emit_norm_T(p, xmid, t, xt, xn, ss, rstd, ident, tp, h, 0,
                    ab[:, 2, :] if isctx else ab[:, 0, :], ab[:, 3, :] if isctx else ab[:, 1, :], tag, "cab" if isctx else "lab")
        p.dma("sp", hx2T[:, :, t * 128:(t + 1) * 128].rearrange("k p t -> p k t"), h[:], reads=[(tag, 0)], writes=[("hx2T", t)], is_output=True, store=True)
        for k in range(32):
            p.op("pe", lambda e, k=k, h=h: e.matmul(lg[:], lhsT=h[:, k, :], rhs=wr[:, k, :], start=(k == 0), stop=(k == 31)),
                 reads=[(tag, 0), "wr"], writes=["lg"])
        p.op("dve", lambda e: e.reduce_max(out=mx[:, 0:1], in_=lg[:], axis=AX.X), reads=["lg"], writes=["mx"])
        p.op("dve", lambda e: e.tensor_scalar(out=mx[:, 0:1], in0=mx[:, 0:1], scalar1=-1.0, scalar2=None, op0=ALU.mult), reads=["mx"], writes=["mx"])
        p.op("act", lambda e: e.activation(out=aff[:], in_=lg[:], func=AF.Exp, bias=mx[:, 0:1], scale=1.0, accum_out=mx[:, 1:2]), reads=["lg", "mx"], writes=["aff", "mx"])
        p.op("dve", lambda e: e.reciprocal(out=mx[:, 1:2], in_=mx[:, 1:2]), reads=["mx"], writes=["mx"])
        p.op("dve", lambda e: e.tensor_scalar(out=aff[:], in0=aff[:], scalar1=mx[:, 1:2], scalar2=None, op0=ALU.mult), reads=["aff", "mx"], writes=["aff"])
        p.op("pe", lambda e: e.transpose(out=tpf[:], in_=aff[:], identity=identf[:]), reads=["aff", "identf"], writes=["tpf"])
        p.op("act", lambda e, t=t: e.activation(out=affT[:, t * 128:(t + 1) * 128], in_=tpf[:], func=AF.Copy), reads=["tpf"], writes=["affT"])

    junk = xt
    for (c0, c1, kk) in ((0, 256, 32), (256, NTOK, 512)):
        n = c1 - c0
        p.op("dve", lambda e: e.memset(st[:, 0:1], 0.0), writes=["st"])
        p.op("dve", lambda e: e.memset(st[:, 1:2], 1.0), reads=["st"], writes=["st"])
        for it in range(niter):
            p.op("dve", lambda e: e.tensor_tensor(out=st[:, 2:3], in0=st[:, 0:1], in1=st[:, 1:2], op=ALU.add), reads=["st"], writes=["st"])
            p.op("dve", lambda e: e.tensor_scalar(out=st[:, 2:3], in0=st[:, 2:3], scalar1=0.5, scalar2=None, op0=ALU.mult), reads=["st"], writes=["st"])
            p.op("dve", lambda e, c0=c0, c1=c1, n=n: e.tensor_scalar(out=junk[0:NEXP, 0:n], in0=affT[:, c0:c1], scalar1=st[:, 2:3], scalar2=None, op0=ALU.is_ge),
                 reads=["affT", "st", "xt"], writes=["xt"])
            p.op("dve", lambda e, n=n: e.reduce_sum(out=st[:, 3:4], in_=junk[0:NEXP, 0:n], axis=AX.X), reads=["xt", "st"], writes=["st"])
            p.op("dve", lambda e, kk=kk: e.tensor_scalar(out=st[:, 4:5], in0=st[:, 3:4], scalar1=float(kk) - 0.5, scalar2=None, op0=ALU.is_ge), reads=["st"], writes=["st"])
            p.op("dve", lambda e: e.tensor_tensor(out=st[:, 5:6], in0=st[:, 2:3], in1=st[:, 0:1], op=ALU.subtract), reads=["st"], writes=["st"])
            p.op("dve", lambda e: e.scalar_tensor_tensor(out=st[:, 0:1], in0=st[:, 5:6], scalar=st[:, 4:5], in1=st[:, 0:1], op0=ALU.mult, op1=ALU.add), reads=["st"], writes=["st"])
            p.op("dve", lambda e: e.tensor_tensor(out=st[:, 5:6], in0=st[:, 1:2], in1=st[:, 2:3], op=ALU.subtract), reads=["st"], writes=["st"])
            p.op("dve", lambda e: e.scalar_tensor_tensor(out=st[:, 1:2], in0=st[:, 5:6], scalar=st[:, 4:5], in1=st[:, 2:3], op0=ALU.mult, op1=ALU.add), reads=["st"], writes=["st"])
        p.op("dve", lambda e, c0=c0, c1=c1: e.tensor_scalar(out=gT[:, c0:c1], in0=affT[:, c0:c1], scalar1=st[:, 0:1], scalar2=None, op0=ALU.is_ge),
             reads=["affT", "st"], writes=["gT"])
        p.op("dve", lambda e, c0=c0, c1=c1: e.tensor_tensor(out=gT[:, c0:c1], in0=gT[:, c0:c1], in1=affT[:, c0:c1], op=ALU.mult), reads=["gT", "affT"], writes=["gT"])
    p.dma("sp", gateT, gT[:], reads=["gT"], writes=["gateT"], is_output=True, store=True)
    return p


def build_ffn(groups=GROUPS4, nexp=NEXP):
    p = Prog()
    xmid = p.dram("xmid", [NTOK, D], F32, "ExternalInput")
    hx2T = p.dram("hx2T", [32, 128, NTOK], BF16, "ExternalInput")
    gateT = p.dram("gateT", [NEXP, NTOK], F32, "ExternalInput")
    wg_d = p.dram("wg_d", [NEXP, D, FF], F32, "ExternalInput")
    wu_d = p.dram("wu_d", [NEXP, D, FF], F32, "ExternalInput")
    wd_d = p.dram("wd_d", [NEXP, FF, D], F32, "ExternalInput")
    gbc = p.dram("gbc", [2, 128, D], F32, "ExternalInput")
    seld = p.dram("seld", [NEXP, NEXP, 128], F32, "ExternalInput")
    xout = p.dram("xout", [NTOK, D], F32, "ExternalOutput")

    hxg = p.sb("hxg", [128, 32, 512], BF16)
    hid = p.sb("hid", [128, 3 * nexp, 512], BF16)
    wg = p.sb("wg", [128, 32, FF], BF16)
    wu = p.sb("wu", [128, 32, FF], BF16)
    wd = p.sb("wd", [128, 3 * nexp, 256], BF16)
    sel = p.sb("sel", [NEXP, NEXP, 128], F32)
    gts = p.sb("gts", [NEXP, 512], F32)
    sg = [p.sb(f"sg{i}", [128, 512], F32) for i in range(2)]
    gt = p.sb("gt", [128, 2, 256], F32)
    xc = [p.sb(f"xc{i}", [128, 256], F32) for i in range(2)]
    ob = [p.sb(f"ob{i}", [128, 256], F32) for i in range(2)]
    gps = [p.ps(f"gps{i}", [128, 512], F32) for i in range(2)]
    ups = [p.ps(f"ups{i}", [128, 512], F32) for i in range(2)]
    bps = p.ps("bps", [128, 512], F32)
    yps = [p.ps(f"yps{i}", [128, 256], F32) for i in range(2)]

    p.dma("sp", sel[:], seld, writes=["sel"])
    ci = 0
    oi = 0
    for g in groups:
        gs = len(g) * 128
        t0 = g[0] * 128
        p.dma("sp", hxg[:, :, 0:gs], hx2T[:, :, t0:t0 + gs].rearrange("k p t -> p k t"), writes=["hxg"])
        p.dma("sp", gts[:, 0:gs], gateT[:, t0:t0 + gs], writes=["gts"])
        for ex in range(nexp):
            for kq in range(4):
                p.dma("pool", wg[:, kq * 8:(kq + 1) * 8, :], wg_d[ex, kq * 1024:(kq + 1) * 1024, :].rearrange("(k p) n -> p k n", p=128), writes=["wg"])
            for kq in range(4):
                p.dma("pool", wu[:, kq * 8:(kq + 1) * 8, :], wu_d[ex, kq * 1024:(kq + 1) * 1024, :].rearrange("(k p) n -> p k n", p=128), writes=["wu"])
            p.op("pe", lambda e, ex=ex, gs=gs: e.matmul(bps[:, 0:gs], lhsT=sel[:, ex, :], rhs=gts[:, 0:gs], start=True, stop=True),
                 reads=["sel", "gts"], writes=["bps"])
            for fc in range(3):
                gp = gps[ci % 2]
                gk = ("gps", ci % 2)
                up = ups[ci % 2]
                uk = ("ups", ci % 2)
                s_ = sg[ci % 2]
                sk = ("sg", ci % 2)
                ci += 1
                for k in range(32):
                    p.op("pe", lambda e, gp=gp, k=k, fc=fc, gs=gs: e.matmul(gp[:, 0:gs], lhsT=wg[:, k, fc * 128:(fc + 1) * 128], rhs=hxg[:, k, 0:gs], start=(k == 0), stop=(k == 31)),
                         reads=["wg", "hxg"], writes=[gk])
                for k in range(32):
                    p.op("pe", lambda e, up=up, k=k, fc=fc, gs=gs: e.matmul(up[:, 0:gs], lhsT=wu[:, k, fc * 128:(fc + 1) * 128], rhs=hxg[:, k, 0:gs], start=(k == 0), stop=(k == 31)),
                         reads=["wu", "hxg"], writes=[uk])
                p.op("act", lambda e, gp=gp, s_=s_, gs=gs: e.activation(out=s_[:, 0:gs], in_=gp[:, 0:gs], func=AF.Silu), reads=[gk], writes=[sk])
                p.op("dve", lambda e, up=up, s_=s_, gs=gs: e.tensor_tensor(out=s_[:, 0:gs], in0=s_[:, 0:gs], in1=up[:, 0:gs], op=ALU.mult), reads=[sk, uk], writes=[sk])
                p.op("dve", lambda e, s_=s_, ex=ex, fc=fc, gs=gs: e.tensor_tensor(out=hid[:, ex * 3 + fc, 0:gs], in0=s_[:, 0:gs], in1=bps[:, 0:gs], op=ALU.mult),
                     reads=[sk, "bps"], writes=["hid"])
        for n in range(16):
            for eq in range(4):
                e0 = eq * (nexp // 4)
                e1 = (eq + 1) * (nexp // 4)
                p.dma("pool", wd[:, e0 * 3:e1 * 3, :], wd_d[e0:e1, :, n * 256:(n + 1) * 256].rearrange("e (c p) n -> p (e c) n", p=128), writes=["wd"])
            p.dma("sp", gt[:], gbc[:, :, n * 256:(n + 1) * 256].rearrange("a p n -> p a n"), writes=["gt"])
            for s, t in enumerate(g):
                y = yps[oi % 2]
                yk = ("yps", oi % 2)
                x_ = xc[oi % 2]
                xk = ("xc", oi % 2)
                o_ = ob[oi % 2]
                ok_ = ("ob", oi % 2)
                oi += 1
                gi = 1 if t < 2 else 0
                p.dma("sp", x_[:], xmid[t * 128:(t + 1) * 128, n * 256:(n + 1) * 256], writes=[xk])
                for j in range(3 * nexp):
                    p.op("pe", lambda e, y=y, j=j, s=s: e.matmul(y[:], lhsT=hid[:, j, s * 128:(s + 1) * 128], rhs=wd[:, j, :], start=(j == 0), stop=(j == 3 * nexp - 1)),
                         reads=["hid", "wd"], writes=[yk])
                p.op("dve", lambda e, y=y, o_=o_, gi=gi: e.tensor_tensor(out=o_[:], in0=y[:], in1=gt[:, gi, :], op=ALU.mult), reads=[yk, "gt"], writes=[ok_])
                p.op("dve", lambda e, o_=o_, x_=x_: e.tensor_tensor(out=o_[:], in0=o_[:], in1=x_[:], op=ALU.add), reads=[ok_, xk], writes=[ok_])
                p.dma("sp", xout[t * 128:(t + 1) * 128, n * 256:(n + 1) * 256], o_[:], reads=[ok_], writes=[("xout", t, n)], is_output=True, store=True)
    return p


def build_final():
    p = Prog()
    xin = p.dram("xin", [SEQ, D], F32, "ExternalInput")
    gbc = p.dram("gbc", [128, D], F32, "ExternalInput")
    out = p.dram("out", [SEQ, D], F32, "ExternalOutput")
    g = p.sb("g", [128, D], F32)
    xt = [p.sb(f"xt{i}", [128, D], F32) for i in range(2)]
    sq = p.sb("sq", [128, D], BF16)
    ss = p.sb("ss", [128, 2], F32)
    p.dma("sp", g[:], gbc, writes=["g"])
    for t in range(SEQ // 128):
        x_ = xt[t % 2]
        xk = ("xt", t % 2)
        sk = ("ss", t % 2)
        c = t % 2
        p.dma("sp", x_[:], xin[t * 128:(t + 1) * 128, :], writes=[xk])
        p.op("act", lambda e, x_=x_, c=c: e.activation(out=sq[:], in_=x_[:], func=AF.Square, accum_out=ss[:, c:c + 1]), reads=[xk], writes=["sq", sk])
        p.op("dve", lambda e, c=c: e.tensor_scalar(out=ss[:, c:c + 1], in0=ss[:, c:c + 1], scalar1=1.0 / D, scalar2=EPS, op0=ALU.mult, op1=ALU.add), reads=[sk], writes=[sk])
        p.op("act", lambda e, c=c: e.activation(out=ss[:, c:c + 1], in_=ss[:, c:c + 1], func=AF.Sqrt), reads=[sk], writes=[sk])
        p.op("dve", lambda e, c=c: e.reciprocal(out=ss[:, c:c + 1], in_=ss[:, c:c + 1]), reads=[sk], writes=[sk])
        p.op("dve", lambda e, x_=x_, c=c: e.scalar_tensor_tensor(out=x_[:], in0=x_[:], scalar=ss[:, c:c + 1], in1=g[:], op0=ALU.mult, op1=ALU.mult),
             reads=[xk, sk, "g"], writes=[xk])
        p.dma("sp", out[t * 128:(t + 1) * 128, :], x_[:], reads=[xk], writes=[("out", t)], is_output=True, store=True)
    return p


def _run2(prog, in_maps):
    nc = prog.finish()
    res = run_bass_kernel_spmd(nc, in_maps, core_ids=[0, 1])
    return res.results


def _bc(v, n=128):
    v = np.asarray(v, np.float32)
    return np.ascontiguousarray(np.broadcast_to(v[None], (n,) + v.shape))


def kernel(x, c, ctx, c_ctx, w_ada, b_ada, norm1_g, norm2_g, w_in, w_out, a_lambda, a_subln_g,
           b_q_norm_g, b_k_norm_g, c_rpb, w_router, w_e_gate, w_e_up, w_e_down, final_g):
    f32 = lambda a: np.asarray(a, np.float32)
    x, c, ctx, c_ctx = f32(x), f32(c), f32(ctx), f32(c_ctx)
    mod = run_ada(c, c_ctx, f32(w_ada), f32(b_ada))
    state = [np.ascontiguousarray(np.concatenate([ctx[b], x[b]], 0)) for b in range(2)]
    ident = np.eye(128, dtype=np.float32)
    rt = rope_tables()
    sel = np.zeros((NEXP, NEXP, 128), np.float32)
    for k in range(NEXP):
        sel[k, k, :] = 1.0
    for l in range(DEPTH):
        sh1, sc1, g1, sh2, sc2, g2 = np.split(mod[l], 6, axis=1)
        lam_init = 0.8 - 0.6 * float(np.exp(-0.3 * l))
        bng = _bc(np.stack([f32(b_q_norm_g[l]), f32(b_k_norm_g[l])]))
        wl_in = np.ascontiguousarray(f32(w_in[l]))
        ims = []
        for b in range(2):
            modv = np.ascontiguousarray(np.stack([fm(norm1_g[l]), fm(sc1[b]), fm(sh1[b]), fm(sc1[2]), fm(sh1[2])], 1))
            ims.append({"xin": state[b], "w_in": wl_in, "modv": modv, "identd": ident, "ropet": rt, "bng": bng})
        ra = _run2(build_inproj(GROUPS9), ims)
        del wl_in
        alam = _bc(f32(a_lambda[l]))
        lconst = _bc(np.array([lam_init, 1.0 - lam_init], np.float32))
        sublg = _bc(f32(a_subln_g[l]))
        cb2 = build_cb2(f32(c_rpb[l]))
        ims = [{"qkT": ra[b]["qkT"], "vtok": ra[b]["vtok"], "alam": alam, "lconst": lconst, "sublg": sublg, "cb2d": cb2} for b in range(2)]
        rb = _run2(build_attn(), ims)
        del ra
        wl_out = np.ascontiguousarray(f32(w_out[l]))
        ims = [{"otok": rb[b]["otok"], "xin": state[b], "w_out": wl_out,
                "gbc": np.ascontiguousarray(np.stack([_bc(g1[b]), _bc(g1[2])])), "identd": ident} for b in range(2)]
        rc = _run2(build_outproj(GROUPS9), ims)
        del rb, wl_out
        xmid = [rc[b]["xmid"] for b in range(2)]
        wr = np.ascontiguousarray(f32(w_router[l]))
        ims = []
        for b in range(2):
            modv = np.ascontiguousarray(np.stack([fm(norm2_g[l]), fm(sc2[b]), fm(sh2[b]), fm(sc2[2]), fm(sh2[2])], 1))
            ims.append({"xmid": xmid[b], "modv": modv, "identd": ident, "w_r": wr})
        rr = _run2(build_router(), ims)
        wg = np.ascontiguousarray(f32(w_e_gate[l]))
        wu = np.ascontiguousarray(f32(w_e_up[l]))
        wd = np.ascontiguousarray(f32(w_e_down[l]))
        ims = [{"xmid": xmid[b], "hx2T": rr[b]["hx2T"], "gateT": rr[b]["gateT"], "wg_d": wg, "wu_d": wu, "wd_d": wd,
                "gbc": np.ascontiguousarray(np.stack([_bc(g2[b]), _bc(g2[2])])), "seld": sel} for b in range(2)]
        rf = _run2(build_ffn(), ims)
        del rr, wg, wu, wd
        state = [rf[b]["xout"] for b in range(2)]
    ims = [{"xin": np.ascontiguousarray(state[b][CTX:]), "gbc": _bc(f32(final_g))} for b in range(2)]
    ro = _run2(build_final(), ims)
    return np.stack([ro[b]["out"] for b in range(2)], 0).astype(np.float32)
```

```python
import contextlib
import numpy as np
import concourse.bass as bass
import concourse.mybir as mybir
from concourse.bass_utils import run_bass_kernel_spmd
from concourse.alu_op_type import AluOpType as ALU

F32 = mybir.dt.float32
BF16 = mybir.dt.bfloat16
AF = mybir.ActivationFunctionType
AX = mybir.AxisListType

NCORES = 8
D = 4096
SEQ = 4096
CTX = 256
DEPTH = 4
IN_W = 10240
NEXP = 16
FF = 384
EPS = 1e-6

SAME_ENGINE_SYNC = True
SEM_LIMIT = 30000


class Prog:
    ENGS = ("pe", "dve", "act", "pool", "sp")

    def __init__(self):
        self.nc = bass.Bass("TRN2", target_bir_lowering=False)
        self.stack = contextlib.ExitStack()
        self.ops = {e: [] for e in self.ENGS}
        self.cur = {}
        self.seen = {e: {} for e in self.ENGS}
        self.lastw = {}
        self.readers = {}
        self.semobj = {}
        self.nsem = 0
        self.out_marks = []

    def dram(self, name, shape, dt, kind):
        return self.nc.dram_tensor(name, list(shape), dt, kind=kind).ap()

    def sb(self, name, shape, dt):
        return self.stack.enter_context(self.nc.sbuf_tensor(name, list(shape), dt))

    def ps(self, name, shape, dt=F32):
        return self.stack.enter_context(self.nc.psum_tensor(name, list(shape), dt))

    def _newsem(self, stream):
        s = self.stack.enter_context(self.nc.semaphore(f"s{self.nsem}"))
        sid = self.nsem
        self.nsem += 1
        self.semobj[sid] = s
        self.cur[stream] = [sid, 0]
        return sid

    def _tick(self, stream, inc):
        if stream not in self.cur or self.cur[stream][1] + inc > SEM_LIMIT:
            self._newsem(stream)
        c = self.cur[stream]
        c[1] += inc
        return (c[0], c[1])

    def _deps(self, eng, reads, writes, own_stream):
        need = {}

        def add(sv):
            if sv is None:
                return
            sid, val = sv
            if need.get(sid, 0) < val:
                need[sid] = val

        for k in reads:
            add(self.lastw.get(k))
        for k in writes:
            add(self.lastw.get(k))
            for sid, val in self.readers.get(k, {}).items():
                add((sid, val))
        waits = []
        seen = self.seen[eng]
        for sid, val in need.items():
            if seen.get(sid, 0) >= val:
                continue
            if own_stream is not None and own_stream in self.cur and self.cur[own_stream][0] == sid:
                if eng == "pe" or not SAME_ENGINE_SYNC:
                    continue
            seen[sid] = val
            waits.append((sid, val))
        return waits

    def _mark(self, tick, reads, writes):
        sid, val = tick
        for k in reads:
            r = self.readers.setdefault(k, {})
            if r.get(sid, 0) < val:
                r[sid] = val
        for k in writes:
            self.lastw[k] = (sid, val)
            self.readers[k] = {}

    def op(self, eng, fn, reads=(), writes=()):
        waits = self._deps(eng, reads, writes, eng)
        tick = self._tick(eng, 1)
        self.ops[eng].append((waits, fn, tick, 1))
        self._mark(tick, reads, writes)

    def dma(self, q, out, in_, reads=(), writes=(), stream=None, is_output=False, store=False, **kw):
        if stream is None:
            stream = ("d", reads[0] if (store or not len(writes)) else writes[0])
        waits = self._deps(q, reads, writes, None)
        tick = self._tick(stream, 16)

        def fn(e, out=out, in_=in_, kw=kw):
            return e.dma_start(out=out, in_=in_, **kw)

        self.ops[q].append((waits, fn, tick, 16))
        self._mark(tick, reads, writes)
        if is_output:
            self.out_marks.append(tick)

    def finish(self):
        fin = {}
        for sid, val in self.out_marks:
            fin[sid] = max(fin.get(sid, 0), val)
        for stream, (sid, val) in self.cur.items():
            pass
        final_waits = list(fin.items())
        nc = self.nc
        ops = self.ops
        semobj = self.semobj

        def emit(e, lst, extra=()):
            for waits, fn, (sid, val), inc in lst:
                for ws, wv in waits:
                    e.wait_ge(semobj[ws], wv)
                ins = fn(e)
                ins.then_inc(semobj[sid], inc)
            for ws, wv in extra:
                e.wait_ge(semobj[ws], wv)

        with nc.Block() as block:
            @block.sync
            def _(e):
                emit(e, ops["sp"], final_waits)

            @block.tensor
            def _(e):
                emit(e, ops["pe"])

            @block.vector
            def _(e):
                emit(e, ops["dve"])

            @block.scalar
            def _(e):
                emit(e, ops["act"])

            @block.gpsimd
            def _(e):
                emit(e, ops["pool"], final_waits)
        self.stack.close()
        return nc


def run(prog, in_maps):
    nc = prog.finish()
    res = run_bass_kernel_spmd(nc, in_maps, core_ids=list(range(NCORES)))
    return res.results


ADA_COLS = 6 * D // NCORES


def build_ada():
    p = Prog()
    c3T = p.dram("c3T", [128, 32, 3], F32, "ExternalInput")
    w = p.dram("w", [DEPTH, D, ADA_COLS], F32, "ExternalInput")
    b = p.dram("b", [DEPTH, 3, ADA_COLS], F32, "ExternalInput")
    out = p.dram("out", [DEPTH, 3, ADA_COLS], F32, "ExternalOutput")

    cT = p.sb("cT", [128, 32, 3], F32)
    sT = p.sb("sT", [128, 32, 3], F32)
    bias = p.sb("bias", [3, DEPTH, ADA_COLS], F32)
    res = p.sb("res", [3, DEPTH, ADA_COLS], F32)
    NB = 3
    KG = 8
    wt = [p.sb(f"wt{i}", [128, KG, 512], F32) for i in range(NB)]
    acc = [p.ps(f"acc{i}", [3, 512], F32) for i in range(2)]

    p.dma("sp", cT[:], c3T, writes=["cT"])
    for l in range(DEPTH):
        p.dma("sp", bias[:, l, :], b[l], writes=[("bias", l)])
    p.op("act", lambda e: e.activation(out=sT[:], in_=cT[:], func=AF.Sigmoid), reads=["cT"], writes=["sT"])
    p.op("dve", lambda e: e.tensor_tensor(out=sT[:], in0=sT[:], in1=cT[:], op=ALU.mult), reads=["sT", "cT"], writes=["sT"])

    it = 0
    grp = 0
    for l in range(DEPTH):
        for n in range(ADA_COLS // 512):
            a = acc[grp % 2]
            ak = ("acc", grp % 2)
            for kg in range(32 // KG):
                t = wt[it % NB]
                tk = ("wt", it % NB)
                q = "sp" if it % 2 == 0 else "act"
                src = w[l, kg * KG * 128:(kg + 1) * KG * 128, n * 512:(n + 1) * 512].rearrange("(k p) n -> p k n", p=128)
                p.dma(q, t[:], src, writes=[tk])
                for kk in range(KG):
                    k = kg * KG + kk
                    p.op("pe", lambda e, a=a, t=t, kk=kk, k=k: e.matmul(a[:], lhsT=sT[:, k, :], rhs=t[:, kk, :], start=(k == 0), stop=(k == 31)),
                         reads=["sT", tk], writes=[ak])
                it += 1
            p.op("dve", lambda e, a=a, l=l, n=n: e.tensor_tensor(out=res[:, l, n * 512:(n + 1) * 512], in0=a[:], in1=bias[:, l, n * 512:(n + 1) * 512], op=ALU.add),
                 reads=[ak, ("bias", l)], writes=[("res", l)])
            grp += 1
        p.dma("sp", out[l], res[:, l, :], reads=[("res", l)], is_output=True)
    return p


def run_ada(c, c_ctx, w_ada, b_ada):
    c3 = np.concatenate([c, c_ctx[None]], axis=0).astype(np.float32)
    c3T = np.ascontiguousarray(c3.T.reshape(32, 128, 3).transpose(1, 0, 2))
    in_maps = []
    for i in range(NCORES):
        sl = slice(i * ADA_COLS, (i + 1) * ADA_COLS)
        in_maps.append({
            "c3T": c3T,
            "w": np.ascontiguousarray(w_ada[:, :, sl]),
            "b": np.ascontiguousarray(np.broadcast_to(b_ada[:, None, sl], (DEPTH, 3, ADA_COLS))),
        })
    res = run(build_ada(), in_maps)
    return np.concatenate([r["out"] for r in res], axis=2)


NT = 34
NTOK = NT * 128
NCH = IN_W // 512
ROPE_A = (0, 1, 2, 3)
ROPE_B = (6, 7, 8, 9)
PLAIN_QK = (11, 12, 13, 14, 15, 16)
V_CH = (4, 5, 10, 17, 18, 19)
VSLOT = {4: 0, 5: 512, 10: 1024, 17: 1536, 18: 2048, 19: 2560}
QSLOT = {0: 0, 1: 4, 2: 8, 3: 12, 6: 16, 7: 20, 8: 24, 9: 28, 11: 32, 12: 36, 13: 40, 14: 44, 15: 48, 16: 52}


def emit_norm_T(p, xsrc, t, xt, xn, ss, rstd, ident, tp, hxT, slot, a_vec, b_vec, tag, abk):
    p.dma("sp", xt[:], xsrc[t * 128:(t + 1) * 128, :], writes=["xt"])
    p.op("act", lambda e: e.activation(out=xn[:], in_=xt[:], func=AF.Square, accum_out=ss[:]),
         reads=["xt"], writes=["xn", "ss"])
    p.op("dve", lambda e: e.tensor_scalar(out=rstd[:], in0=ss[:], scalar1=1.0 / D, scalar2=EPS, op0=ALU.mult, op1=ALU.add),
         reads=["ss"], writes=["rstd"])
    p.op("act", lambda e: e.activation(out=rstd[:], in_=rstd[:], func=AF.Sqrt), reads=["rstd"], writes=["rstd"])
    p.op("dve", lambda e: e.reciprocal(out=rstd[:], in_=rstd[:]), reads=["rstd"], writes=["rstd"])
    p.op("act", lambda e: e.activation(out=xn[:], in_=xt[:], func=AF.Copy, scale=rstd[:, 0:1]),
         reads=["xt", "rstd"], writes=["xn"])
    for k4 in range(8):
        tpk = ("tp", k4 % 2)
        tpt = tp[k4 % 2]
        for j in range(4):
            k = k4 * 4 + j
            p.op("pe", lambda e, k=k, j=j, tpt=tpt: e.transpose(out=tpt[:, j, :], in_=xn[:, k * 128:(k + 1) * 128], identity=ident[:]),
                 reads=["xn", "ident"], writes=[tpk])
        for j in range(4):
            k = k4 * 4 + j
            p.op("dve", lambda e, k=k, j=j, tpt=tpt: e.tensor_scalar(
                out=hxT[:, k, slot * 128:(slot + 1) * 128], in0=tpt[:, j, :],
                scalar1=a_vec[:, k:k + 1], scalar2=b_vec[:, k:k + 1], op0=ALU.mult, op1=ALU.add),
                reads=[tpk, abk], writes=[(tag, slot)])


def build_inproj(groups, out_kind="ExternalOutput"):
    p = Prog()
    GMAX = max(len(g) for g in groups)
    xin = p.dram("xin", [NTOK, D], F32, "ExternalInput")
    w_in = p.dram("w_in", [D, IN_W], F32, "ExternalInput")
    modv = p.dram("modv", [128, 5, 32], F32, "ExternalInput")
    identd = p.dram("identd", [128, 128], F32, "ExternalInput")
    ropet = p.dram("ropet", [NT, 128, 4, 128], F32, "ExternalInput")
    bng = p.dram("bng", [128, 2, 128], F32, "ExternalInput")
    qkT = p.dram("qkT", [56, 128, NTOK], BF16, out_kind)
    vtok = p.dram("vtok", [NTOK, 3072], BF16, out_kind)

    ident = p.sb("ident", [128, 128], BF16)
    mv = p.sb("mv", [128, 5, 32], F32)
    ab = p.sb("ab", [128, 4, 32], F32)
    bn = p.sb("bn", [128, 2, 128], F32)
    xt = p.sb("xt", [128, D], F32)
    xn = p.sb("xn", [128, D], BF16)
    ss = p.sb("ss", [128, 1], F32)
    rstd = p.sb("rstd", [128, 1], F32)
    hxT = p.sb("hxT", [128, 32, GMAX * 128], BF16)
    wb = [p.sb(f"wb{i}", [128, 32, 512], BF16) for i in range(2)]
    rt = [p.sb(f"rt{i}", [128, 4, 128], F32) for i in range(2)]
    t1 = p.sb("t1", [128, 512], F32)
    t2 = p.sb("t2", [128, 512], F32)
    nrm = p.sb("nrm", [128, 512], F32)
    raw = p.sb("raw", [128, 512], F32)
    ss4 = p.sb("ss4", [128, 4], F32)
    pp = [p.sb(f"pp{i}", [128, 512], BF16) for i in range(2)]
    ppT = [p.sb(f"ppT{i}", [128, 4, 128], BF16) for i in range(2)]
    tp = [p.ps(f"tp{i}", [128, 4, 128], BF16) for i in range(2)]
    acc = [p.ps(f"acc{i}", [128, 512], F32) for i in range(2)]

    p.dma("pool", ident[:], identd, writes=["ident"])
    p.dma("sp", mv[:], modv, writes=["mv"])
    p.dma("sp", bn[:], bng, writes=["bn"])
    for i, (sc, sh) in enumerate(((1, 2), (3, 4))):
        p.op("dve", lambda e, i=i, sc=sc: e.scalar_tensor_tensor(out=ab[:, 2 * i, :], in0=mv[:, sc, :], scalar=1.0, in1=mv[:, 0, :], op0=ALU.add, op1=ALU.mult),
             reads=["mv"], writes=["lab" if i == 0 else "cab"])
        p.op("dve", lambda e, i=i, sh=sh: e.tensor_copy(out=ab[:, 2 * i + 1, :], in_=mv[:, sh, :]),
             reads=["mv"], writes=["lab" if i == 0 else "cab"])

    ppi = 0
    wi = 0
    for g in groups:
        for s, t in enumerate(g):
            isctx = t < 2
            emit_norm_T(p, xin, t, xt, xn, ss, rstd, ident, tp, hxT, s,
                        ab[:, 2, :] if isctx else ab[:, 0, :], ab[:, 3, :] if isctx else ab[:, 1, :],
                        "hx", "cab" if isctx else "lab")
        for c in range(NCH):
            w = wb[wi % 2]
            wk = ("wb", wi % 2)
            wi += 1
            for kq in range(4):
                p.dma("pool", w[:, kq * 8:(kq + 1) * 8, :],
                      w_in[kq * 1024:(kq + 1) * 1024, c * 512:(c + 1) * 512].rearrange("(k p) n -> p k n", p=128),
                      writes=[wk])
            for s, t in enumerate(g):
                a = acc[(s) % 2]
                ak = ("acc", s % 2)
                for k in range(32):
                    p.op("pe", lambda e, a=a, w=w, k=k, s=s: e.matmul(a[:], lhsT=hxT[:, k, s * 128:(s + 1) * 128], rhs=w[:, k, :], start=(k == 0), stop=(k == 31)),
                         reads=[("hx", s), wk], writes=[ak])
                o = pp[ppi % 2]
                ok = ("pp", ppi % 2)
                oT = ppT[ppi % 2]
                oTk = ("ppT", ppi % 2)
                ppi += 1
                if c in V_CH or c in PLAIN_QK:
                    p.op("act", lambda e, o=o, a=a: e.activation(out=o[:], in_=a[:], func=AF.Copy), reads=[ak], writes=[ok])
                else:
                    isB = c in ROPE_B
                    r = rt[ppi % 2]
                    rk = ("rt", ppi % 2)
                    p.dma("sp", r[:], ropet[t], writes=[rk])
                    src = a
                    srck = ak
                    if isB:
                        gi = 0 if c < 9 else 1
                        for h in range(4):
                            p.op("act", lambda e, h=h, a=a: e.activation(
                                out=t1[:, h * 128:(h + 1) * 128], in_=a[:, h * 128:(h + 1) * 128], func=AF.Square, accum_out=ss4[:, h:h + 1]),
                                reads=[ak], writes=["t1", "ss4"])
                        p.op("dve", lambda e: e.tensor_scalar(out=ss4[:], in0=ss4[:], scalar1=1.0 / 128, scalar2=EPS, op0=ALU.mult, op1=ALU.add),
                             reads=["ss4"], writes=["ss4"])
                        p.op("act", lambda e: e.activation(out=ss4[:], in_=ss4[:], func=AF.Sqrt), reads=["ss4"], writes=["ss4"])
                        p.op("dve", lambda e: e.reciprocal(out=ss4[:], in_=ss4[:]), reads=["ss4"], writes=["ss4"])
                        for h in range(4):
                            p.op("dve", lambda e, h=h, gi=gi, a=a: e.scalar_tensor_tensor(
                                out=nrm[:, h * 128:(h + 1) * 128], in0=a[:, h * 128:(h + 1) * 128], scalar=ss4[:, h:h + 1],
                                in1=bn[:, gi, :], op0=ALU.mult, op1=ALU.mult),
                                reads=[ak, "ss4", "bn"], writes=["nrm"])
                        src = nrm
                        srck = "nrm"
                    ci, si = (2, 3) if isB else (0, 1)
                    hw = 32 if isB else 16
                    nb = 128 // (2 * hw)
                    Cb = r[:, ci, :].unsqueeze(1).broadcast_to([128, 4, 128])
                    sv = src[:].rearrange("p (h b two w) -> p h b two w", h=4, b=nb, two=2, w=hw)
                    Sv = r[:, si, :].rearrange("p (b two w) -> p b two w", b=nb, two=2, w=hw)
                    t2v = t2[:].rearrange("p (h b two w) -> p h b two w", h=4, b=nb, two=2, w=hw)
                    p.op("dve", lambda e, src=src, Cb=Cb: e.tensor_tensor(out=t1[:].rearrange("p (h d) -> p h d", h=4), in0=src[:].rearrange("p (h d) -> p h d", h=4), in1=Cb, op=ALU.mult),
                         reads=[srck, rk], writes=["t1"])
                    for h in range(4):
                        for half in range(2):
                            p.op("dve", lambda e, h=h, half=half, sv=sv, Sv=Sv, t2v=t2v: e.tensor_tensor(
                                out=t2v[:, h, :, half, :], in0=sv[:, h, :, 1 - half, :], in1=Sv[:, :, half, :], op=ALU.mult),
                                reads=[srck, rk], writes=["t2"])
                    p.op("dve", lambda e, o=o: e.tensor_tensor(out=o[:], in0=t1[:], in1=t2[:], op=ALU.add),
                         reads=["t1", "t2"], writes=[ok])
                if c in V_CH:
                    p.dma("sp", vtok[t * 128:(t + 1) * 128, VSLOT[c]:VSLOT[c] + 512], o[:], reads=[ok], writes=[("vtok", t, c)],
                          is_output=(out_kind == "ExternalOutput"), store=True)
                else:
                    tpt = tp[ppi % 2]
                    tpk = ("tp", ppi % 2)
                    for h in range(4):
                        p.op("pe", lambda e, h=h, o=o, tpt=tpt: e.transpose(out=tpt[:, h, :], in_=o[:, h * 128:(h + 1) * 128], identity=ident[:]),
                             reads=[ok, "ident"], writes=[tpk])
                    p.op("act", lambda e, oT=oT, tpt=tpt: e.activation(out=oT[:], in_=tpt[:], func=AF.Copy), reads=[tpk], writes=[oTk])
                    p.dma("sp", qkT[QSLOT[c]:QSLOT[c] + 4, :, t * 128:(t + 1) * 128].rearrange("h d t -> d h t"), oT[:], reads=[oTk],
                          writes=[("qkT", t, c)], is_output=(out_kind == "ExternalOutput"), store=True)
    return p


def rope_tables():
    n = np.arange(SEQ)
    row = (n // 64).astype(np.float64)
    col = (n % 64).astype(np.float64)
    out = np.zeros((NT, 128, 4, 128), np.float32)
    out[0:2, :, 0, :] = 1.0
    out[0:2, :, 2, :] = 1.0

    def blk(pos, half):
        fr = 10000.0 ** (-np.arange(half, dtype=np.float32) / half)
        ang = pos.astype(np.float32)[:, None] * fr[None, :]
        c, s_ = np.cos(ang), np.sin(ang)
        return np.concatenate([c, c], 1), np.concatenate([-s_, s_], 1)

    cr, sr = blk(row, 16)
    cc, sc = blk(col, 16)
    CA = np.concatenate([cr, cc, cr, cc], 1)
    SA = np.concatenate([sr, sc, sr, sc], 1)
    cr, sr = blk(row, 32)
    cc, sc = blk(col, 32)
    CB = np.concatenate([cr, cc], 1)
    SB = np.concatenate([sr, sc], 1)
    lat = np.stack([CA, SA, CB, SB], 1).reshape(32, 128, 4, 128)
    out[2:] = lat
    return out


def fm(v):
    return np.ascontiguousarray(np.asarray(v, np.float32).reshape(32, 128).T)


NEG = -30000.0


LAY_FULL = {
    "A": [(h, 8 + h, 128 * h, 128 * h) for h in range(8)],
    "B": [(16 + h, 28 + h // 3, 1024 + 128 * (h // 3), 1024 + 128 * h) for h in range(12)],
    "C": [(32 + h, 44 + h, 1536 + 128 * h, 2560 + 128 * h, h) for h in range(12)],
    "nq": 56, "vw": 3072, "ow": D, "ncb": 12,
}
LAY_Q4 = {
    "A": [(j, 2 + j, 128 * j, 128 * j) for j in range(2)],
    "B": [(4 + j, 7, 256, 256 + 128 * j) for j in range(3)],
    "C": [(8 + j, 11 + j, 384 + 128 * j, 640 + 128 * j, j) for j in range(3)],
    "nq": 14, "vw": 768, "ow": 1024, "ncb": 3,
}


def build_attn(heads=None, qblocks=None, crows=None, in_kind="ExternalInput", out_kind="ExternalOutput", lay=None):
    lay = lay or LAY_FULL
    p = Prog()
    qkT = p.dram("qkT", [lay["nq"], 128, NTOK], BF16, in_kind)
    vtok = p.dram("vtok", [NTOK, lay["vw"]], BF16, in_kind)
    alam = p.dram("alam", [128, 4, 64], F32, "ExternalInput")
    lconst = p.dram("lconst", [128, 2], F32, "ExternalInput")
    sublg = p.dram("sublg", [128, 128], F32, "ExternalInput")
    cb2d = p.dram("cb2d", [lay["ncb"], 128, 14, 64], F32, "ExternalInput")
    otok = p.dram("otok", [NTOK, lay["ow"]], BF16, out_kind)

    KT = p.sb("KT", [128, NTOK], BF16)
    QT = p.sb("QT", [128, NTOK], BF16)
    V1 = p.sb("V1", [128, NT, 129], BF16)
    V1o = p.sb("V1o", [128, 31, 129], BF16)
    cb2 = p.sb("cb2", [128, 14, 64], F32)
    al = p.sb("al", [128, 4, 64], F32)
    lc = p.sb("lc", [128, 2], F32)
    sg = p.sb("sg", [128, 128], F32)
    lam = p.sb("lam", [128, 4], F32)
    junk = p.sb("junk", [128, 128], F32)
    Pt = [p.sb(f"Pt{i}", [128, 512], BF16) for i in range(2)]
    sc = [p.sb(f"sc{i}", [128, 64], F32) for i in range(2)]
    o1 = p.sb("o1", [128, 4, 128], F32)
    o2 = p.sb("o2", [128, 4, 128], F32)
    rz = p.sb("rz", [128, 4], F32)
    ssq = p.sb("ssq", [128, 4], F32)
    ost = [p.sb(f"ost{i}", [128, 4, 128], BF16) for i in range(2)]
    pss = [p.ps(f"pss{i}", [128, 512], F32) for i in range(2)]
    acc = [p.ps(f"pacc{i}", [128, 512], F32) for i in range(4)]

    p.dma("sp", al[:], alam, writes=["al"])
    p.dma("sp", lc[:], lconst, writes=["lc"])
    p.dma("sp", sg[:], sublg, writes=["sg"])
    p.op("dve", lambda e: e.memset(V1[:, :, 128:129], 1.0), writes=["V1ones"])
    p.op("dve", lambda e: e.memset(V1o[:, :, 128:129], 1.0), writes=["V1oones"])
    for i in range(2):
        p.op("dve", lambda e, i=i: e.tensor_tensor(out=junk[:, 0:64], in0=al[:, 2 * i, :], in1=al[:, 2 * i + 1, :], op=ALU.mult),
             reads=["al"], writes=["junk"])
        p.op("dve", lambda e, i=i: e.reduce_sum(out=lam[:, i:i + 1], in_=junk[:, 0:64], axis=AX.X), reads=["junk"], writes=["lam"])
    p.op("act", lambda e: e.activation(out=lam[:, 0:2], in_=lam[:, 0:2], func=AF.Exp), reads=["lam"], writes=["lam"])
    p.op("dve", lambda e: e.tensor_tensor(out=lam[:, 2:3], in0=lam[:, 0:1], in1=lam[:, 1:2], op=ALU.subtract), reads=["lam"], writes=["lam"])
    p.op("dve", lambda e: e.tensor_tensor(out=lam[:, 2:3], in0=lam[:, 2:3], in1=lc[:, 0:1], op=ALU.add), reads=["lam", "lc"], writes=["lam"])
    p.op("dve", lambda e: e.tensor_scalar(out=lam[:, 3:4], in0=lam[:, 2:3], scalar1=-1.0, scalar2=None, op0=ALU.mult), reads=["lam"], writes=["lam"])
    p.op("dve", lambda e: e.tensor_scalar(out=sg[:], in0=sg[:], scalar1=lc[:, 1:2], scalar2=None, op0=ALU.mult), reads=["sg", "lc"], writes=["sg"])

    cnt = {"s": 0, "o": 0}

    def load_head(qh, kh, vcol, need_odd):
        p.dma("sp", QT[:], qkT[qh], writes=["QT"])
        p.dma("sp", KT[:], qkT[kh], writes=["KT"])
        p.dma("sp", V1[:, :, 0:128], vtok[:, vcol:vcol + 128].rearrange("(j p) d -> p j d", p=128), writes=["V1"])
        if need_odd:
            p.dma("sp", V1o[:, :, 0:128], vtok[320:320 + 31 * 128, vcol:vcol + 128].rearrange("(j p) d -> p j d", p=128), writes=["V1o"])

    def dense_pass(kr, q0, nq, ktiles, scale, fin):
        nqt = nq // 128
        for idx, j in enumerate(ktiles):
            s_ = pss[cnt["s"] % 2]
            sk = ("pss", cnt["s"] % 2)
            pt = Pt[cnt["s"] % 2]
            pk = ("Pt", cnt["s"] % 2)
            cnt["s"] += 1
            p.op("pe", lambda e, s_=s_, j=j: e.matmul(s_[:, 0:nq], lhsT=KT[kr[0]:kr[1], j * 128:(j + 1) * 128], rhs=QT[kr[0]:kr[1], q0:q0 + nq], start=True, stop=True),
                 reads=["KT", "QT"], writes=[sk])
            p.op("act", lambda e, s_=s_, pt=pt: e.activation(out=pt[:, 0:nq], in_=s_[:, 0:nq], func=AF.Exp, scale=scale), reads=[sk], writes=[pk])
            for qi in range(nqt):
                p.op("pe", lambda e, qi=qi, pt=pt, j=j, idx=idx: e.matmul(acc[qi][:, 0:129], lhsT=pt[:, qi * 128:(qi + 1) * 128], rhs=V1[:, j, :],
                                                                   start=(idx == 0), stop=(idx == len(ktiles) - 1)),
                     reads=[pk, "V1", "V1ones"], writes=[("acc", qi)])
        for qi in range(nqt):
            fin(qi)

    def fin_to(dst, dk):
        def f(qi):
            p.op("dve", lambda e, qi=qi: e.reciprocal(out=rz[:, qi:qi + 1], in_=acc[qi][:, 128:129]), reads=[("acc", qi)], writes=[("rz", qi)])
            p.op("act", lambda e, qi=qi: e.activation(out=dst[:, qi, :], in_=acc[qi][:, 0:128], func=AF.Copy, scale=rz[:, qi:qi + 1]),
                 reads=[("acc", qi), ("rz", qi)], writes=[(dk, qi)])
        return f

    def store(o, ok_, q0, nqt, col):
        p.dma("sp", otok[q0:q0 + nqt * 128, col:col + 128].rearrange("(j p) d -> p j d", p=128), o[:, 0:nqt, :],
              reads=[(ok_, qi) for qi in range(nqt)], writes=[("otok", q0, col)], is_output=(out_kind == "ExternalOutput"), store=True,
              stream=("d", ok_))

    blocks = [(0, 256, [0, 1])] + [(256 + 512 * i, 512, list(range(NT))) for i in range(8)]
    if qblocks is not None:
        blocks = [blocks[i] for i in qblocks]
    allheads = [(g_, h) for g_ in ("A", "B", "C") for h in range(len(lay[g_]))]
    if heads is not None:
        allheads = heads
    for grp, h in allheads:
        if grp == "A":
            load_head(lay["A"][h][0], lay["A"][h][1], lay["A"][h][2], False)
            col = lay["A"][h][3]
            for (q0, nq, kts) in blocks:
                nqt = nq // 128
                o = ost[cnt["o"] % 2]
                ok_ = ("ost", cnt["o"] % 2)
                cnt["o"] += 1
                dense_pass((0, 64), q0, nq, kts, 0.125, fin_to(o1, "o1"))
                dense_pass((64, 128), q0, nq, kts, 0.125, fin_to(o2, "o2"))
                for qi in range(nqt):
                    p.op("dve", lambda e, qi=qi: e.scalar_tensor_tensor(out=o1[:, qi, :], in0=o2[:, qi, :], scalar=lam[:, 3:4], in1=o1[:, qi, :], op0=ALU.mult, op1=ALU.add),
                         reads=[("o1", qi), ("o2", qi), "lam"], writes=[("o1", qi)])
                    p.op("act", lambda e, qi=qi: e.activation(out=junk[:], in_=o1[:, qi, :], func=AF.Square, accum_out=ssq[:, qi:qi + 1]),
                         reads=[("o1", qi)], writes=["junk", ("ssq", qi)])
                    p.op("dve", lambda e, qi=qi: e.tensor_scalar(out=ssq[:, qi:qi + 1], in0=ssq[:, qi:qi + 1], scalar1=1.0 / 128, scalar2=EPS, op0=ALU.mult, op1=ALU.add),
                         reads=[("ssq", qi)], writes=[("ssq", qi)])
                    p.op("act", lambda e, qi=qi: e.activation(out=ssq[:, qi:qi + 1], in_=ssq[:, qi:qi + 1], func=AF.Sqrt), reads=[("ssq", qi)], writes=[("ssq", qi)])
                    p.op("dve", lambda e, qi=qi: e.reciprocal(out=ssq[:, qi:qi + 1], in_=ssq[:, qi:qi + 1]), reads=[("ssq", qi)], writes=[("ssq", qi)])
                    p.op("dve", lambda e, qi=qi, o=o: e.scalar_tensor_tensor(out=o[:, qi, :], in0=o1[:, qi, :], scalar=ssq[:, qi:qi + 1], in1=sg[:], op0=ALU.mult, op1=ALU.mult),
                         reads=[("o1", qi), ("ssq", qi), "sg"], writes=[(ok_, qi)])
                store(o, ok_, q0, nqt, col)
        elif grp == "B":
            load_head(lay["B"][h][0], lay["B"][h][1], lay["B"][h][2], False)
            col = lay["B"][h][3]
            sc_ = 128 ** -0.5
            for (q0, nq, kts) in blocks:
                nqt = nq // 128
                o = ost[cnt["o"] % 2]
                ok_ = ("ost", cnt["o"] % 2)
                cnt["o"] += 1
                dense_pass((0, 128), q0, nq, kts, sc_, fin_to(o, ok_))
                store(o, ok_, q0, nqt, col)
        else:
            load_head(lay["C"][h][0], lay["C"][h][1], lay["C"][h][2], True)
            p.dma("sp", cb2[:], cb2d[lay["C"][h][4]], writes=["cb2"])
            col = lay["C"][h][3]
            sc_ = 128 ** -0.5
            if qblocks is None or 0 in qblocks:
                o = ost[cnt["o"] % 2]
                ok_ = ("ost", cnt["o"] % 2)
                cnt["o"] += 1
                dense_pass((0, 128), 0, 256, [0, 1], sc_, fin_to(o, ok_))
                store(o, ok_, 0, 2, col)
            rows = range(64) if crows is None else crows
            for r in rows:
                rs = min(max(r - 4, 0), 56)
                q0 = 256 + 64 * r
                half = r % 2
                if half == 0 or r == rows[0]:
                    o = ost[cnt["o"] % 2]
                    ok_ = ("ost", cnt["o"] % 2)
                    cnt["o"] += 1
                a = acc[r % 4]
                akey = ("acc", r % 4)
                kts = [("c", 0), ("c", 1)] + [("l", i) for i in range(4)]
                for idx, (kind, i) in enumerate(kts):
                    s_ = pss[cnt["s"] % 2]
                    sk = ("pss", cnt["s"] % 2)
                    pt = Pt[cnt["s"] % 2]
                    pk = ("Pt", cnt["s"] % 2)
                    scb = sc[cnt["s"] % 2]
                    sck = ("sc", cnt["s"] % 2)
                    cnt["s"] += 1
                    if kind == "c":
                        k0 = i * 128
                        vt = V1[:, i, :]
                        vk = "V1"
                    else:
                        row0 = rs + 2 * i
                        k0 = 256 + 64 * row0
                        if row0 % 2 == 0:
                            vt = V1[:, 2 + row0 // 2, :]
                            vk = "V1"
                        else:
                            vt = V1o[:, (row0 - 1) // 2, :]
                            vk = "V1o"
                    p.op("pe", lambda e, s_=s_, k0=k0, q0=q0: e.matmul(s_[:, 0:64], lhsT=KT[:, k0:k0 + 128], rhs=QT[:, q0:q0 + 64], start=True, stop=True),
                         reads=["KT", "QT"], writes=[sk])
                    if kind == "c":
                        p.op("act", lambda e, s_=s_, pt=pt: e.activation(out=pt[:, 0:64], in_=s_[:, 0:64], func=AF.Exp, scale=sc_), reads=[sk], writes=[pk])
                    else:
                        di = rs + 2 * i - r + 7
                        p.op("dve", lambda e, s_=s_, scb=scb, di=di: e.scalar_tensor_tensor(out=scb[:], in0=s_[:, 0:64], scalar=sc_, in1=cb2[:, di, :], op0=ALU.mult, op1=ALU.add),
                             reads=[sk, "cb2"], writes=[sck])
                        p.op("act", lambda e, scb=scb, pt=pt: e.activation(out=pt[:, 0:64], in_=scb[:], func=AF.Exp), reads=[sck], writes=[pk])
                    p.op("pe", lambda e, a=a, pt=pt, vt=vt, idx=idx: e.matmul(a[0:64, 0:129], lhsT=pt[:, 0:64], rhs=vt, start=(idx == 0), stop=(idx == 5)),
                         reads=[pk, vk, vk + "ones"], writes=[akey])
                p.op("dve", lambda e, a=a, r=r: e.reciprocal(out=rz[0:64, r % 4:r % 4 + 1], in_=a[0:64, 128:129]), reads=[akey], writes=[("rz", r % 4)])
                p.op("act", lambda e, a=a, r=r, o=o, half=half: e.activation(out=o[0:64, half * 2, :], in_=a[0:64, 0:128], func=AF.Copy, scale=rz[0:64, r % 4:r % 4 + 1]),
                     reads=[akey, ("rz", r % 4)], writes=[(ok_, half)])
                p.dma("sp", otok[q0:q0 + 64, col:col + 128], o[0:64, half * 2, :], reads=[(ok_, half)], writes=[("otok", q0, col)],
                      is_output=(out_kind == "ExternalOutput"), store=True, stream=("d", ok_, half))
    return p


def build_cb2(rpb):
    qc = np.arange(64)
    cs = np.clip(qc - 8, 0, 48)
    kc = np.arange(64)
    valid = (kc[:, None] >= cs[None, :]) & (kc[:, None] < cs[None, :] + 16)
    dc = np.clip(kc[:, None] - qc[None, :] + 15, 0, 30)
    out = np.full((12, 128, 14, 64), NEG, np.float32)
    for di in range(14):
        for kr in range(2):
            v = rpb[:, di + kr][:, dc]
            out[:, kr * 64:(kr + 1) * 64, di, :] = np.where(valid[None], v, NEG)
    return out


GROUPS4 = [[0, 1]] + [list(range(2 + 4 * i, 6 + 4 * i)) for i in range(8)]
GROUPS9 = [list(range(0, 9)), list(range(9, 18)), list(range(18, 26)), list(range(26, 34))]


def build_outproj(groups=GROUPS9):
    p = Prog()
    otok = p.dram("otok", [NTOK, D], BF16, "ExternalInput")
    xin = p.dram("xin", [NTOK, D], F32, "ExternalInput")
    w_out = p.dram("w_out", [D, D], F32, "ExternalInput")
    gbc = p.dram("gbc", [2, 128, D], F32, "ExternalInput")
    identd = p.dram("identd", [128, 128], F32, "ExternalInput")
    xmid = p.dram("xmid", [NTOK, D], F32, "ExternalOutput")
    GMAX = max(len(g) for g in groups)
    ident = p.sb("ident", [128, 128], BF16)
    ot = p.sb("ot", [128, D], BF16)
    OT = p.sb("OT", [128, 32, GMAX * 128], BF16)
    wb = [p.sb(f"wb{i}", [128, 32, 512], BF16) for i in range(2)]
    gt = p.sb("gt", [128, 2, 512], F32)
    xc = [p.sb(f"xc{i}", [128, 512], F32) for i in range(2)]
    ob = [p.sb(f"ob{i}", [128, 512], F32) for i in range(2)]
    tp = [p.ps(f"tp{i}", [128, 4, 128], BF16) for i in range(2)]
    acc = [p.ps(f"acc{i}", [128, 512], F32) for i in range(2)]
    p.dma("pool", ident[:], identd, writes=["ident"])
    wi = 0
    oi = 0
    for g in groups:
        for s, t in enumerate(g):
            p.dma("sp", ot[:], otok[t * 128:(t + 1) * 128, :], writes=["ot"])
            for k4 in range(8):
                tpt = tp[k4 % 2]
                tpk = ("tp", k4 % 2)
                for j in range(4):
                    k = k4 * 4 + j
                    p.op("pe", lambda e, k=k, j=j, tpt=tpt: e.transpose(out=tpt[:, j, :], in_=ot[:, k * 128:(k + 1) * 128], identity=ident[:]),
                         reads=["ot", "ident"], writes=[tpk])
                p.op("act", lambda e, k4=k4, s=s, tpt=tpt: e.activation(out=OT[:, k4 * 4:(k4 + 1) * 4, s * 128:(s + 1) * 128], in_=tpt[:], func=AF.Copy),
                     reads=[tpk], writes=[("OT", s)])
        for n in range(8):
            w = wb[wi % 2]
            wk = ("wb", wi % 2)
            wi += 1
            for kq in range(4):
                p.dma("pool", w[:, kq * 8:(kq + 1) * 8, :],
                      w_out[kq * 1024:(kq + 1) * 1024, n * 512:(n + 1) * 512].rearrange("(k p) n -> p k n", p=128), writes=[wk])
            p.dma("sp", gt[:], gbc[:, :, n * 512:(n + 1) * 512].rearrange("a p n -> p a n"), writes=["gt"])
            for s, t in enumerate(g):
                a = acc[oi % 2]
                ak = ("acc", oi % 2)
                x_ = xc[oi % 2]
                xk = ("xc", oi % 2)
                o_ = ob[oi % 2]
                ok_ = ("ob", oi % 2)
                oi += 1
                gi = 1 if t < 2 else 0
                p.dma("sp", x_[:], xin[t * 128:(t + 1) * 128, n * 512:(n + 1) * 512], writes=[xk])
                for k in range(32):
                    p.op("pe", lambda e, a=a, w=w, k=k, s=s: e.matmul(a[:], lhsT=OT[:, k, s * 128:(s + 1) * 128], rhs=w[:, k, :], start=(k == 0), stop=(k == 31)),
                         reads=[("OT", s), wk], writes=[ak])
                p.op("dve", lambda e, a=a, o_=o_, gi=gi: e.tensor_tensor(out=o_[:], in0=a[:], in1=gt[:, gi, :], op=ALU.mult), reads=[ak, "gt"], writes=[ok_])
                p.op("dve", lambda e, o_=o_, x_=x_: e.tensor_tensor(out=o_[:], in0=o_[:], in1=x_[:], op=ALU.add), reads=[ok_, xk], writes=[ok_])
                p.dma("sp", xmid[t * 128:(t + 1) * 128, n * 512:(n + 1) * 512], o_[:], reads=[ok_], writes=[("xmid", t, n)], is_output=True, store=True)
    return p


def build_router(niter=30):
    p = Prog()
    xmid = p.dram("xmid", [NTOK, D], F32, "ExternalInput")
    modv = p.dram("modv", [128, 5, 32], F32, "ExternalInput")
    identd = p.dram("identd", [128, 128], F32, "ExternalInput")
    w_r = p.dram("w_r", [D, NEXP], F32, "ExternalInput")
    hx2T = p.dram("hx2T", [32, 128, NTOK], BF16, "ExternalOutput")
    gateT = p.dram("gateT", [NEXP, NTOK], F32, "ExternalOutput")

    ident = p.sb("ident", [128, 128], BF16)
    identf = p.sb("identf", [128, 128], F32)
    mv = p.sb("mv", [128, 5, 32], F32)
    ab = p.sb("ab", [128, 4, 32], F32)
    wr = p.sb("wr", [128, 32, NEXP], BF16)
    xt = p.sb("xt", [128, D], F32)
    xn = p.sb("xn", [128, D], BF16)
    ss = p.sb("ss", [128, 1], F32)
    rstd = p.sb("rstd", [128, 1], F32)
    hT = [p.sb(f"hT{i}", [128, 32, 128], BF16) for i in range(2)]
    aff = p.sb("aff", [128, NEXP], F32)
    mx = p.sb("mx", [128, 2], F32)
    affT = p.sb("affT", [NEXP, NTOK], F32)
    gT = p.sb("gT", [NEXP, NTOK], F32)
    st = p.sb("st", [NEXP, 8], F32)
    tp = [p.ps(f"tp{i}", [128, 4, 128], BF16) for i in range(2)]
    lg = p.ps("lg", [128, NEXP], F32)
    tpf = p.ps("tpf", [NEXP, 128], F32)

    p.dma("pool", ident[:], identd, writes=["ident"])
    p.dma("sp", identf[:], identd, writes=["identf"])
    p.dma("sp", mv[:], modv, writes=["mv"])
    p.dma("pool", wr[:], w_r.rearrange("(k p) n -> p k n", p=128), writes=["wr"])
    for i, (sc, sh) in enumerate(((1, 2), (3, 4))):
        p.op("dve", lambda e, i=i, sc=sc: e.scalar_tensor_tensor(out=ab[:, 2 * i, :], in0=mv[:, sc, :], scalar=1.0, in1=mv[:, 0, :], op0=ALU.add, op1=ALU.mult),
             reads=["mv"], writes=["lab" if i == 0 else "cab"])
        p.op("dve", lambda e, i=i, sh=sh: e.tensor_copy(out=ab[:, 2 * i + 1, :], in_=mv[:, sh, :]), reads=["mv"], writes=["lab" if i == 0 else "cab"])

    for t in range(NT):
        isctx = t < 2
        h = hT[t % 2]
        tag = "hT%d" % (t % 2)
        emit_norm_T(p, xmid, t, xt, xn, ss, rstd, ident, tp, h, 0,
                    ab[:, 2, :] if isctx else ab[:, 0, :], ab[:, 3, :] if isctx else ab[:, 1, :], tag, "cab" if isctx else "lab")
        p.dma("sp", hx2T[:, :, t * 128:(t + 1) * 128].rearrange("k p t -> p k t"), h[:], reads=[(tag, 0)], writes=[("hx2T", t)], is_output=True, store=True)
        for k in range(32):
            p.op("pe", lambda e, k=k, h=h: e.matmul(lg[:], lhsT=h[:, k, :], rhs=wr[:, k, :], start=(k == 0), stop=(k == 31)),
                 reads=[(tag, 0), "wr"], writes=["lg"])
        p.op("dve", lambda e: e.reduce_max(out=mx[:, 0:1], in_=lg[:], axis=AX.X), reads=["lg"], writes=["mx"])
        p.op("dve", lambda e: e.tensor_scalar(out=mx[:, 0:1], in0=mx[:, 0:1], scalar1=-1.0, scalar2=None, op0=ALU.mult), reads=["mx"], writes=["mx"])
        p.op("act", lambda e: e.activation(out=aff[:], in_=lg[:], func=AF.Exp, bias=mx[:, 0:1], scale=1.0, accum_out=mx[:, 1:2]), reads=["lg", "mx"], writes=["aff", "mx"])
        p.op("dve", lambda e: e.reciprocal(out=mx[:, 1:2], in_=mx[:, 1:2]), reads=["mx"], writes=["mx"])
        p.op("dve", lambda e: e.tensor_scalar(out=aff[:], in0=aff[:], scalar1=mx[:, 1:2], scalar2=None, op0=ALU.mult), reads=["aff", "mx"], writes=["aff"])
        p.op("pe", lambda e: e.transpose(out=tpf[:], in_=aff[:], identity=identf[:]), reads=["aff", "identf"], writes=["tpf"])
        p.op("act", lambda e, t=t: e.activation(out=affT[:, t * 128:(t + 1) * 128], in_=tpf[:], func=AF.Copy), reads=["tpf"], writes=["affT"])

    junk = xt
    for (c0, c1, kk) in ((0, 256, 32), (256, NTOK, 512)):
        n = c1 - c0
        p.op("dve", lambda e: e.memset(st[:, 0:1], 0.0), writes=["st"])
        p.op("dve", lambda e: e.memset(st[:, 1:2], 1.0), reads=["st"], writes=["st"])
        for it in range(niter):
            p.op("dve", lambda e: e.tensor_tensor(out=st[:, 2:3], in0=st[:, 0:1], in1=st[:, 1:2], op=ALU.add), reads=["st"], writes=["st"])
            p.op("dve", lambda e: e.tensor_scalar(out=st[:, 2:3], in0=st[:, 2:3], scalar1=0.5, scalar2=None, op0=ALU.mult), reads=["st"], writes=["st"])
            p.op("dve", lambda e, c0=c0, c1=c1, n=n: e.tensor_scalar(out=junk[0:NEXP, 0:n], in0=affT[:, c0:c1], scalar1=st[:, 2:3], scalar2=None, op0=ALU.is_ge),
                 reads=["affT", "st", "xt"], writes=["xt"])
            p.op("dve", lambda e, n=n: e.reduce_sum(out=st[:, 3:4], in_=junk[0:NEXP, 0:n], axis=AX.X), reads=["xt", "st"], writes=["st"])
            p.op("dve", lambda e, kk=kk: e.tensor_scalar(out=st[:, 4:5], in0=st[:, 3:4], scalar1=float(kk) - 0.5, scalar2=None, op0=ALU.is_ge), reads=["st"], writes=["st"])
            p.op("dve", lambda e: e.tensor_tensor(out=st[:, 5:6], in0=st[:, 2:3], in1=st[:, 0:1], op=ALU.subtract), reads=["st"], writes=["st"])
            p.op("dve", lambda e: e.scalar_tensor_tensor(out=st[:, 0:1], in0=st[:, 5:6], scalar=st[:, 4:5], in1=st[:, 0:1], op0=ALU.mult, op1=ALU.add), reads=["st"], writes=["st"])
            p.op("dve", lambda e: e.tensor_tensor(out=st[:, 5:6], in0=st[:, 1:2], in1=st[:, 2:3], op=ALU.subtract), reads=["st"], writes=["st"])
            p.op("dve", lambda e: e.scalar_tensor_tensor(out=st[:, 1:2], in0=st[:, 5:6], scalar=st[:, 4:5], in1=st[:, 2:3], op0=ALU.mult, op1=ALU.add), reads=["st"], writes=["st"])
        p.op("dve", lambda e, c0=c0, c1=c1: e.tensor_scalar(out=gT[:, c0:c1], in0=affT[:, c0:c1], scalar1=st[:, 0:1], scalar2=None, op0=ALU.is_ge),
             reads=["affT", "st"], writes=["gT"])
        p.op("dve", lambda e, c0=c0, c1=c1: e.tensor_tensor(out=gT[:, c0:c1], in0=gT[:, c0:c1], in1=affT[:, c0:c1], op=ALU.mult), reads=["gT", "affT"], writes=["gT"])
    p.dma("sp", gateT, gT[:], reads=["gT"], writes=["gateT"], is_output=True, store=True)
    return p


def build_ffn(groups=GROUPS4, nexp=NEXP):
    p = Prog()
    xmid = p.dram("xmid", [NTOK, D], F32, "ExternalInput")
    hx2T = p.dram("hx2T", [32, 128, NTOK], BF16, "ExternalInput")
    gateT = p.dram("gateT", [NEXP, NTOK], F32, "ExternalInput")
    wg_d = p.dram("wg_d", [NEXP, D, FF], F32, "ExternalInput")
    wu_d = p.dram("wu_d", [NEXP, D, FF], F32, "ExternalInput")
    wd_d = p.dram("wd_d", [NEXP, FF, D], F32, "ExternalInput")
    gbc = p.dram("gbc", [2, 128, D], F32, "ExternalInput")
    seld = p.dram("seld", [NEXP, NEXP, 128], F32, "ExternalInput")
    xout = p.dram("xout", [NTOK, D], F32, "ExternalOutput")

    hxg = p.sb("hxg", [128, 32, 512], BF16)
    hid = p.sb("hid", [128, 3 * nexp, 512], BF16)
    wg = p.sb("wg", [128, 32, FF], BF16)
    wu = p.sb("wu", [128, 32, FF], BF16)
    wd = p.sb("wd", [128, 3 * nexp, 256], BF16)
    sel = p.sb("sel", [NEXP, NEXP, 128], F32)
    gts = p.sb("gts", [NEXP, 512], F32)
    sg = [p.sb(f"sg{i}", [128, 512], F32) for i in range(2)]
    gt = p.sb("gt", [128, 2, 256], F32)
    xc = [p.sb(f"xc{i}", [128, 256], F32) for i in range(2)]
    ob = [p.sb(f"ob{i}", [128, 256], F32) for i in range(2)]
    gps = [p.ps(f"gps{i}", [128, 512], F32) for i in range(2)]
    ups = [p.ps(f"ups{i}", [128, 512], F32) for i in range(2)]
    bps = p.ps("bps", [128, 512], F32)
    yps = [p.ps(f"yps{i}", [128, 256], F32) for i in range(2)]

    p.dma("sp", sel[:], seld, writes=["sel"])
    ci = 0
    oi = 0
    for g in groups:
        gs = len(g) * 128
        t0 = g[0] * 128
        p.dma("sp", hxg[:, :, 0:gs], hx2T[:, :, t0:t0 + gs].rearrange("k p t -> p k t"), writes=["hxg"])
        p.dma("sp", gts[:, 0:gs], gateT[:, t0:t0 + gs], writes=["gts"])
        for ex in range(nexp):
            for kq in range(4):
                p.dma("pool", wg[:, kq * 8:(kq + 1) * 8, :], wg_d[ex, kq * 1024:(kq + 1) * 1024, :].rearrange("(k p) n -> p k n", p=128), writes=["wg"])
            for kq in range(4):
                p.dma("pool", wu[:, kq * 8:(kq + 1) * 8, :], wu_d[ex, kq * 1024:(kq + 1) * 1024, :].rearrange("(k p) n -> p k n", p=128), writes=["wu"])
            p.op("pe", lambda e, ex=ex, gs=gs: e.matmul(bps[:, 0:gs], lhsT=sel[:, ex, :], rhs=gts[:, 0:gs], start=True, stop=True),
                 reads=["sel", "gts"], writes=["bps"])
            for fc in range(3):
                gp = gps[ci % 2]
                gk = ("gps", ci % 2)
                up = ups[ci % 2]
                uk = ("ups", ci % 2)
                s_ = sg[ci % 2]
                sk = ("sg", ci % 2)
                ci += 1
                for k in range(32):
                    p.op("pe", lambda e, gp=gp, k=k, fc=fc, gs=gs: e.matmul(gp[:, 0:gs], lhsT=wg[:, k, fc * 128:(fc + 1) * 128], rhs=hxg[:, k, 0:gs], start=(k == 0), stop=(k == 31)),
                         reads=["wg", "hxg"], writes=[gk])
                for k in range(32):
                    p.op("pe", lambda e, up=up, k=k, fc=fc, gs=gs: e.matmul(up[:, 0:gs], lhsT=wu[:, k, fc * 128:(fc + 1) * 128], rhs=hxg[:, k, 0:gs], start=(k == 0), stop=(k == 31)),
                         reads=["wu", "hxg"], writes=[uk])
                p.op("act", lambda e, gp=gp, s_=s_, gs=gs: e.activation(out=s_[:, 0:gs], in_=gp[:, 0:gs], func=AF.Silu), reads=[gk], writes=[sk])
                p.op("dve", lambda e, up=up, s_=s_, gs=gs: e.tensor_tensor(out=s_[:, 0:gs], in0=s_[:, 0:gs], in1=up[:, 0:gs], op=ALU.mult), reads=[sk, uk], writes=[sk])
                p.op("dve", lambda e, s_=s_, ex=ex, fc=fc, gs=gs: e.tensor_tensor(out=hid[:, ex * 3 + fc, 0:gs], in0=s_[:, 0:gs], in1=bps[:, 0:gs], op=ALU.mult),
                     reads=[sk, "bps"], writes=["hid"])
        for n in range(16):
            for eq in range(4):
                e0 = eq * (nexp // 4)
                e1 = (eq + 1) * (nexp // 4)
                p.dma("pool", wd[:, e0 * 3:e1 * 3, :], wd_d[e0:e1, :, n * 256:(n + 1) * 256].rearrange("e (c p) n -> p (e c) n", p=128), writes=["wd"])
            p.dma("sp", gt[:], gbc[:, :, n * 256:(n + 1) * 256].rearrange("a p n -> p a n"), writes=["gt"])
            for s, t in enumerate(g):
                y = yps[oi % 2]
                yk = ("yps", oi % 2)
                x_ = xc[oi % 2]
                xk = ("xc", oi % 2)
                o_ = ob[oi % 2]
                ok_ = ("ob", oi % 2)
                oi += 1
                gi = 1 if t < 2 else 0
                p.dma("sp", x_[:], xmid[t * 128:(t + 1) * 128, n * 256:(n + 1) * 256], writes=[xk])
                for j in range(3 * nexp):
                    p.op("pe", lambda e, y=y, j=j, s=s: e.matmul(y[:], lhsT=hid[:, j, s * 128:(s + 1) * 128], rhs=wd[:, j, :], start=(j == 0), stop=(j == 3 * nexp - 1)),
                         reads=["hid", "wd"], writes=[yk])
                p.op("dve", lambda e, y=y, o_=o_, gi=gi: e.tensor_tensor(out=o_[:], in0=y[:], in1=gt[:, gi, :], op=ALU.mult), reads=[yk, "gt"], writes=[ok_])
                p.op("dve", lambda e, o_=o_, x_=x_: e.tensor_tensor(out=o_[:], in0=o_[:], in1=x_[:], op=ALU.add), reads=[ok_, xk], writes=[ok_])
                p.dma("sp", xout[t * 128:(t + 1) * 128, n * 256:(n + 1) * 256], o_[:], reads=[ok_], writes=[("xout", t, n)], is_output=True, store=True)
    return p


def build_final():
    p = Prog()
    xin = p.dram("xin", [SEQ, D], F32, "ExternalInput")
    gbc = p.dram("gbc", [128, D], F32, "ExternalInput")
    out = p.dram("out", [SEQ, D], F32, "ExternalOutput")
    g = p.sb("g", [128, D], F32)
    xt = [p.sb(f"xt{i}", [128, D], F32) for i in range(2)]
    sq = p.sb("sq", [128, D], BF16)
    ss = p.sb("ss", [128, 2], F32)
    p.dma("sp", g[:], gbc, writes=["g"])
    for t in range(SEQ // 128):
        x_ = xt[t % 2]
        xk = ("xt", t % 2)
        sk = ("ss", t % 2)
        c = t % 2
        p.dma("sp", x_[:], xin[t * 128:(t + 1) * 128, :], writes=[xk])
        p.op("act", lambda e, x_=x_, c=c: e.activation(out=sq[:], in_=x_[:], func=AF.Square, accum_out=ss[:, c:c + 1]), reads=[xk], writes=["sq", sk])
        p.op("dve", lambda e, c=c: e.tensor_scalar(out=ss[:, c:c + 1], in0=ss[:, c:c + 1], scalar1=1.0 / D, scalar2=EPS, op0=ALU.mult, op1=ALU.add), reads=[sk], writes=[sk])
        p.op("act", lambda e, c=c: e.activation(out=ss[:, c:c + 1], in_=ss[:, c:c + 1], func=AF.Sqrt), reads=[sk], writes=[sk])
        p.op("dve", lambda e, c=c: e.reciprocal(out=ss[:, c:c + 1], in_=ss[:, c:c + 1]), reads=[sk], writes=[sk])
        p.op("dve", lambda e, x_=x_, c=c: e.scalar_tensor_tensor(out=x_[:], in0=x_[:], scalar=ss[:, c:c + 1], in1=g[:], op0=ALU.mult, op1=ALU.mult),
             reads=[xk, sk, "g"], writes=[xk])
        p.dma("sp", out[t * 128:(t + 1) * 128, :], x_[:], reads=[xk], writes=[("out", t)], is_output=True, store=True)
    return p


def _run2(prog, in_maps):
    nc = prog.finish()
    res = run_bass_kernel_spmd(nc, in_maps, core_ids=[0, 1])
    return res.results


def _bc(v, n=128):
    v = np.asarray(v, np.float32)
    return np.ascontiguousarray(np.broadcast_to(v[None], (n,) + v.shape))


def kernel(x, c, ctx, c_ctx, w_ada, b_ada, norm1_g, norm2_g, w_in, w_out, a_lambda, a_subln_g,
           b_q_norm_g, b_k_norm_g, c_rpb, w_router, w_e_gate, w_e_up, w_e_down, final_g):
    f32 = lambda a: np.asarray(a, np.float32)
    x, c, ctx, c_ctx = f32(x), f32(c), f32(ctx), f32(c_ctx)
    mod = run_ada(c, c_ctx, f32(w_ada), f32(b_ada))
    state = [np.ascontiguousarray(np.concatenate([ctx[b], x[b]], 0)) for b in range(2)]
    ident = np.eye(128, dtype=np.float32)
    rt = rope_tables()
    sel = np.zeros((NEXP, NEXP, 128), np.float32)
    for k in range(NEXP):
        sel[k, k, :] = 1.0
    for l in range(DEPTH):
        sh1, sc1, g1, sh2, sc2, g2 = np.split(mod[l], 6, axis=1)
        lam_init = 0.8 - 0.6 * float(np.exp(-0.3 * l))
        bng = _bc(np.stack([f32(b_q_norm_g[l]), f32(b_k_norm_g[l])]))
        wl_in = np.ascontiguousarray(f32(w_in[l]))
        ims = []
        for b in range(2):
            modv = np.ascontiguousarray(np.stack([fm(norm1_g[l]), fm(sc1[b]), fm(sh1[b]), fm(sc1[2]), fm(sh1[2])], 1))
            ims.append({"xin": state[b], "w_in": wl_in, "modv": modv, "identd": ident, "ropet": rt, "bng": bng})
        ra = _run2(build_inproj(GROUPS9), ims)
        del wl_in
        alam = _bc(f32(a_lambda[l]))
        lconst = _bc(np.array([lam_init, 1.0 - lam_init], np.float32))
        sublg = _bc(f32(a_subln_g[l]))
        cb2 = build_cb2(f32(c_rpb[l]))
        ims = []
        for b in range(2):
            qk, vt = ra[b]["qkT"], ra[b]["vtok"]
            for g in range(4):
                qs = ([2 * g + j for j in range(2)] + [8 + 2 * g + j for j in range(2)] + [16 + 3 * g + j for j in range(3)] + [28 + g]
                      + [32 + 3 * g + j for j in range(3)] + [44 + 3 * g + j for j in range(3)])
                vc = [2 * g + j for j in range(2)] + [8 + g] + [12 + 3 * g + j for j in range(3)]
                vsel = np.concatenate([np.arange(128 * v, 128 * v + 128) for v in vc])
                ims.append({"qkT": np.ascontiguousarray(qk[qs]), "vtok": np.ascontiguousarray(vt[:, vsel]), "alam": alam, "lconst": lconst,
                            "sublg": sublg, "cb2d": np.ascontiguousarray(cb2[3 * g:3 * g + 3])})
        r8 = run_bass_kernel_spmd(build_attn(lay=LAY_Q4).finish(), ims, core_ids=list(range(8))).results
        rb = []
        for b in range(2):
            o = np.empty((NTOK, D), dtype=r8[0]["otok"].dtype)
            for g in range(4):
                oc = r8[4 * b + g]["otok"]
                for j in range(2):
                    o[:, 128 * (2 * g + j):128 * (2 * g + j) + 128] = oc[:, 128 * j:128 * j + 128]
                for j in range(3):
                    o[:, 1024 + 128 * (3 * g + j):1024 + 128 * (3 * g + j) + 128] = oc[:, 256 + 128 * j:256 + 128 * j + 128]
                    o[:, 2560 + 128 * (3 * g + j):2560 + 128 * (3 * g + j) + 128] = oc[:, 640 + 128 * j:640 + 128 * j + 128]
            rb.append({"otok": o})
        del ra
        wl_out = np.ascontiguousarray(f32(w_out[l]))
        ims = [{"otok": rb[b]["otok"], "xin": state[b], "w_out": wl_out,
                "gbc": np.ascontiguousarray(np.stack([_bc(g1[b]), _bc(g1[2])])), "identd": ident} for b in range(2)]
        rc = _run2(build_outproj(GROUPS9), ims)
        del rb, wl_out
        xmid = [rc[b]["xmid"] for b in range(2)]
        wr = np.ascontiguousarray(f32(w_router[l]))
        ims = []
        for b in range(2):
            modv = np.ascontiguousarray(np.stack([fm(norm2_g[l]), fm(sc2[b]), fm(sh2[b]), fm(sc2[2]), fm(sh2[2])], 1))
            ims.append({"xmid": xmid[b], "modv": modv, "identd": ident, "w_r": wr})
        rr = _run2(build_router(), ims)
        wg = np.ascontiguousarray(f32(w_e_gate[l]))
        wu = np.ascontiguousarray(f32(w_e_up[l]))
        wd = np.ascontiguousarray(f32(w_e_down[l]))
        ims = [{"xmid": xmid[b], "hx2T": rr[b]["hx2T"], "gateT": rr[b]["gateT"], "wg_d": wg, "wu_d": wu, "wd_d": wd,
                "gbc": np.ascontiguousarray(np.stack([_bc(g2[b]), _bc(g2[2])])), "seld": sel} for b in range(2)]
        rf = _run2(build_ffn(), ims)
        del rr, wg, wu, wd
        state = [rf[b]["xout"] for b in range(2)]
    ims = [{"xin": np.ascontiguousarray(state[b][CTX:]), "gbc": _bc(f32(final_g))} for b in range(2)]
    ro = _run2(build_final(), ims)
    return np.stack([ro[b]["out"] for b in range(2)], 0).astype(np.float32)
```

```python
import contextlib
import numpy as np
import concourse.bass as bass
import concourse.mybir as mybir
from concourse.bass_utils import run_bass_kernel_spmd
from concourse.alu_op_type import AluOpType as ALU

F32 = mybir.dt.float32
BF16 = mybir.dt.bfloat16
AF = mybir.ActivationFunctionType
AX = mybir.AxisListType

NCORES = 8
D = 4096
SEQ = 4096
CTX = 256
DEPTH = 4
IN_W = 10240
NEXP = 16
FF = 384
EPS = 1e-6

SAME_ENGINE_SYNC = True
SEM_LIMIT = 30000


class Prog:
    ENGS = ("pe", "dve", "act", "pool", "sp")

    def __init__(self):
        self.nc = bass.Bass("TRN2", target_bir_lowering=False)
        self.stack = contextlib.ExitStack()
        self.ops = {e: [] for e in self.ENGS}
        self.cur = {}
        self.seen = {e: {} for e in self.ENGS}
        self.lastw = {}
        self.readers = {}
        self.semobj = {}
        self.nsem = 0
        self.out_marks = []

    def dram(self, name, shape, dt, kind):
        return self.nc.dram_tensor(name, list(shape), dt, kind=kind).ap()

    def sb(self, name, shape, dt):
        return self.stack.enter_context(self.nc.sbuf_tensor(name, list(shape), dt))

    def ps(self, name, shape, dt=F32):
        return self.stack.enter_context(self.nc.psum_tensor(name, list(shape), dt))

    def _newsem(self, stream):
        s = self.stack.enter_context(self.nc.semaphore(f"s{self.nsem}"))
        sid = self.nsem
        self.nsem += 1
        self.semobj[sid] = s
        self.cur[stream] = [sid, 0]
        return sid

    def _tick(self, stream, inc):
        if stream not in self.cur or self.cur[stream][1] + inc > SEM_LIMIT:
            self._newsem(stream)
        c = self.cur[stream]
        c[1] += inc
        return (c[0], c[1])

    def _deps(self, eng, reads, writes, own_stream):
        need = {}

        def add(sv):
            if sv is None:
                return
            sid, val = sv
            if need.get(sid, 0) < val:
                need[sid] = val

        for k in reads:
            add(self.lastw.get(k))
        for k in writes:
            add(self.lastw.get(k))
            for sid, val in self.readers.get(k, {}).items():
                add((sid, val))
        waits = []
        seen = self.seen[eng]
        for sid, val in need.items():
            if seen.get(sid, 0) >= val:
                continue
            if own_stream is not None and own_stream in self.cur and self.cur[own_stream][0] == sid:
                if eng == "pe" or not SAME_ENGINE_SYNC:
                    continue
            seen[sid] = val
            waits.append((sid, val))
        return waits

    def _mark(self, tick, reads, writes):
        sid, val = tick
        for k in reads:
            r = self.readers.setdefault(k, {})
            if r.get(sid, 0) < val:
                r[sid] = val
        for k in writes:
            self.lastw[k] = (sid, val)
            self.readers[k] = {}

    def op(self, eng, fn, reads=(), writes=()):
        waits = self._deps(eng, reads, writes, eng)
        tick = self._tick(eng, 1)
        self.ops[eng].append((waits, fn, tick, 1))
        self._mark(tick, reads, writes)

    def dma(self, q, out, in_, reads=(), writes=(), stream=None, is_output=False, store=False, **kw):
        if stream is None:
            stream = ("d", reads[0] if (store or not len(writes)) else writes[0])
        waits = self._deps(q, reads, writes, None)
        tick = self._tick(stream, 16)

        def fn(e, out=out, in_=in_, kw=kw):
            return e.dma_start(out=out, in_=in_, **kw)

        self.ops[q].append((waits, fn, tick, 16))
        self._mark(tick, reads, writes)
        if is_output:
            self.out_marks.append(tick)

    def finish(self):
        fin = {}
        for sid, val in self.out_marks:
            fin[sid] = max(fin.get(sid, 0), val)
        for stream, (sid, val) in self.cur.items():
            pass
        final_waits = list(fin.items())
        nc = self.nc
        ops = self.ops
        semobj = self.semobj

        def emit(e, lst, extra=()):
            for waits, fn, (sid, val), inc in lst:
                for ws, wv in waits:
                    e.wait_ge(semobj[ws], wv)
                ins = fn(e)
                ins.then_inc(semobj[sid], inc)
            for ws, wv in extra:
                e.wait_ge(semobj[ws], wv)

        with nc.Block() as block:
            @block.sync
            def _(e):
                emit(e, ops["sp"], final_waits)

            @block.tensor
            def _(e):
                emit(e, ops["pe"])

            @block.vector
            def _(e):
                emit(e, ops["dve"])

            @block.scalar
            def _(e):
                emit(e, ops["act"])

            @block.gpsimd
            def _(e):
                emit(e, ops["pool"], final_waits)
        self.stack.close()
        return nc


def run(prog, in_maps):
    nc = prog.finish()
    res = run_bass_kernel_spmd(nc, in_maps, core_ids=list(range(NCORES)))
    return res.results


ADA_COLS = 6 * D // NCORES


def build_ada():
    p = Prog()
    c3T = p.dram("c3T", [128, 32, 3], F32, "ExternalInput")
    w = p.dram("w", [DEPTH, D, ADA_COLS], F32, "ExternalInput")
    b = p.dram("b", [DEPTH, 3, ADA_COLS], F32, "ExternalInput")
    out = p.dram("out", [DEPTH, 3, ADA_COLS], F32, "ExternalOutput")

    cT = p.sb("cT", [128, 32, 3], F32)
    sT = p.sb("sT", [128, 32, 3], F32)
    bias = p.sb("bias", [3, DEPTH, ADA_COLS], F32)
    res = p.sb("res", [3, DEPTH, ADA_COLS], F32)
    NB = 3
    KG = 8
    wt = [p.sb(f"wt{i}", [128, KG, 512], F32) for i in range(NB)]
    acc = [p.ps(f"acc{i}", [3, 512], F32) for i in range(2)]

    p.dma("sp", cT[:], c3T, writes=["cT"])
    for l in range(DEPTH):
        p.dma("sp", bias[:, l, :], b[l], writes=[("bias", l)])
    p.op("act", lambda e: e.activation(out=sT[:], in_=cT[:], func=AF.Sigmoid), reads=["cT"], writes=["sT"])
    p.op("dve", lambda e: e.tensor_tensor(out=sT[:], in0=sT[:], in1=cT[:], op=ALU.mult), reads=["sT", "cT"], writes=["sT"])

    it = 0
    grp = 0
    for l in range(DEPTH):
        for n in range(ADA_COLS // 512):
            a = acc[grp % 2]
            ak = ("acc", grp % 2)
            for kg in range(32 // KG):
                t = wt[it % NB]
                tk = ("wt", it % NB)
                q = "sp" if it % 2 == 0 else "act"
                src = w[l, kg * KG * 128:(kg + 1) * KG * 128, n * 512:(n + 1) * 512].rearrange("(k p) n -> p k n", p=128)
                p.dma(q, t[:], src, writes=[tk])
                for kk in range(KG):
                    k = kg * KG + kk
                    p.op("pe", lambda e, a=a, t=t, kk=kk, k=k: e.matmul(a[:], lhsT=sT[:, k, :], rhs=t[:, kk, :], start=(k == 0), stop=(k == 31)),
                         reads=["sT", tk], writes=[ak])
                it += 1
            p.op("dve", lambda e, a=a, l=l, n=n: e.tensor_tensor(out=res[:, l, n * 512:(n + 1) * 512], in0=a[:], in1=bias[:, l, n * 512:(n + 1) * 512], op=ALU.add),
                 reads=[ak, ("bias", l)], writes=[("res", l)])
            grp += 1
        p.dma("sp", out[l], res[:, l, :], reads=[("res", l)], is_output=True)
    return p


def run_ada(c, c_ctx, w_ada, b_ada):
    c3 = np.concatenate([c, c_ctx[None]], axis=0).astype(np.float32)
    c3T = np.ascontiguousarray(c3.T.reshape(32, 128, 3).transpose(1, 0, 2))
    in_maps = []
    for i in range(NCORES):
        sl = slice(i * ADA_COLS, (i + 1) * ADA_COLS)
        in_maps.append({
            "c3T": c3T,
            "w": np.ascontiguousarray(w_ada[:, :, sl]),
            "b": np.ascontiguousarray(np.broadcast_to(b_ada[:, None, sl], (DEPTH, 3, ADA_COLS))),
        })
    res = run(build_ada(), in_maps)
    return np.concatenate([r["out"] for r in res], axis=2)


NT = 34
NTOK = NT * 128
NCH = IN_W // 512
ROPE_A = (0, 1, 2, 3)
ROPE_B = (6, 7, 8, 9)
PLAIN_QK = (11, 12, 13, 14, 15, 16)
V_CH = (4, 5, 10, 17, 18, 19)
VSLOT = {4: 0, 5: 512, 10: 1024, 17: 1536, 18: 2048, 19: 2560}
QSLOT = {0: 0, 1: 4, 2: 8, 3: 12, 6: 16, 7: 20, 8: 24, 9: 28, 11: 32, 12: 36, 13: 40, 14: 44, 15: 48, 16: 52}


def emit_norm_T(p, xsrc, t, xt, xn, ss, rstd, ident, tp, hxT, slot, a_vec, b_vec, tag, abk):
    p.dma("sp", xt[:], xsrc[t * 128:(t + 1) * 128, :], writes=["xt"])
    p.op("act", lambda e: e.activation(out=xn[:], in_=xt[:], func=AF.Square, accum_out=ss[:]),
         reads=["xt"], writes=["xn", "ss"])
    p.op("dve", lambda e: e.tensor_scalar(out=rstd[:], in0=ss[:], scalar1=1.0 / D, scalar2=EPS, op0=ALU.mult, op1=ALU.add),
         reads=["ss"], writes=["rstd"])
    p.op("act", lambda e: e.activation(out=rstd[:], in_=rstd[:], func=AF.Sqrt), reads=["rstd"], writes=["rstd"])
    p.op("dve", lambda e: e.reciprocal(out=rstd[:], in_=rstd[:]), reads=["rstd"], writes=["rstd"])
    p.op("act", lambda e: e.activation(out=xn[:], in_=xt[:], func=AF.Copy, scale=rstd[:, 0:1]),
         reads=["xt", "rstd"], writes=["xn"])
    for k4 in range(8):
        tpk = ("tp", k4 % 2)
        tpt = tp[k4 % 2]
        for j in range(4):
            k = k4 * 4 + j
            p.op("pe", lambda e, k=k, j=j, tpt=tpt: e.transpose(out=tpt[:, j, :], in_=xn[:, k * 128:(k + 1) * 128], identity=ident[:]),
                 reads=["xn", "ident"], writes=[tpk])
        for j in range(4):
            k = k4 * 4 + j
            p.op("dve", lambda e, k=k, j=j, tpt=tpt: e.tensor_scalar(
                out=hxT[:, k, slot * 128:(slot + 1) * 128], in0=tpt[:, j, :],
                scalar1=a_vec[:, k:k + 1], scalar2=b_vec[:, k:k + 1], op0=ALU.mult, op1=ALU.add),
                reads=[tpk, abk], writes=[(tag, slot)])


def build_inproj(groups, out_kind="ExternalOutput", ntok=NTOK, nt=NT, ctx_tiles=(0, 1)):
    p = Prog()
    GMAX = max(len(g) for g in groups)
    xin = p.dram("xin", [ntok, D], F32, "ExternalInput")
    w_in = p.dram("w_in", [D, IN_W], F32, "ExternalInput")
    modv = p.dram("modv", [128, 5, 32], F32, "ExternalInput")
    identd = p.dram("identd", [128, 128], F32, "ExternalInput")
    ropet = p.dram("ropet", [nt, 128, 4, 128], F32, "ExternalInput")
    bng = p.dram("bng", [128, 2, 128], F32, "ExternalInput")
    qkT = p.dram("qkT", [56, 128, ntok], BF16, out_kind)
    vtok = p.dram("vtok", [ntok, 3072], BF16, out_kind)

    ident = p.sb("ident", [128, 128], BF16)
    mv = p.sb("mv", [128, 5, 32], F32)
    ab = p.sb("ab", [128, 4, 32], F32)
    bn = p.sb("bn", [128, 2, 128], F32)
    xt = p.sb("xt", [128, D], F32)
    xn = p.sb("xn", [128, D], BF16)
    ss = p.sb("ss", [128, 1], F32)
    rstd = p.sb("rstd", [128, 1], F32)
    hxT = p.sb("hxT", [128, 32, GMAX * 128], BF16)
    wb = [p.sb(f"wb{i}", [128, 32, 512], BF16) for i in range(2)]
    rt = [p.sb(f"rt{i}", [128, 4, 128], F32) for i in range(2)]
    t1 = p.sb("t1", [128, 512], F32)
    t2 = p.sb("t2", [128, 512], F32)
    nrm = p.sb("nrm", [128, 512], F32)
    raw = p.sb("raw", [128, 512], F32)
    ss4 = p.sb("ss4", [128, 4], F32)
    pp = [p.sb(f"pp{i}", [128, 512], BF16) for i in range(2)]
    ppT = [p.sb(f"ppT{i}", [128, 4, 128], BF16) for i in range(2)]
    tp = [p.ps(f"tp{i}", [128, 4, 128], BF16) for i in range(2)]
    acc = [p.ps(f"acc{i}", [128, 512], F32) for i in range(2)]

    p.dma("pool", ident[:], identd, writes=["ident"])
    p.dma("sp", mv[:], modv, writes=["mv"])
    p.dma("sp", bn[:], bng, writes=["bn"])
    for i, (sc, sh) in enumerate(((1, 2), (3, 4))):
        p.op("dve", lambda e, i=i, sc=sc: e.scalar_tensor_tensor(out=ab[:, 2 * i, :], in0=mv[:, sc, :], scalar=1.0, in1=mv[:, 0, :], op0=ALU.add, op1=ALU.mult),
             reads=["mv"], writes=["lab" if i == 0 else "cab"])
        p.op("dve", lambda e, i=i, sh=sh: e.tensor_copy(out=ab[:, 2 * i + 1, :], in_=mv[:, sh, :]),
             reads=["mv"], writes=["lab" if i == 0 else "cab"])

    ppi = 0
    wi = 0
    for g in groups:
        for s, t in enumerate(g):
            isctx = t in ctx_tiles
            emit_norm_T(p, xin, t, xt, xn, ss, rstd, ident, tp, hxT, s,
                        ab[:, 2, :] if isctx else ab[:, 0, :], ab[:, 3, :] if isctx else ab[:, 1, :],
                        "hx", "cab" if isctx else "lab")
        for c in range(NCH):
            w = wb[wi % 2]
            wk = ("wb", wi % 2)
            wi += 1
            for kq in range(4):
                p.dma("pool", w[:, kq * 8:(kq + 1) * 8, :],
                      w_in[kq * 1024:(kq + 1) * 1024, c * 512:(c + 1) * 512].rearrange("(k p) n -> p k n", p=128),
                      writes=[wk])
            for s, t in enumerate(g):
                a = acc[(s) % 2]
                ak = ("acc", s % 2)
                for k in range(32):
                    p.op("pe", lambda e, a=a, w=w, k=k, s=s: e.matmul(a[:], lhsT=hxT[:, k, s * 128:(s + 1) * 128], rhs=w[:, k, :], start=(k == 0), stop=(k == 31)),
                         reads=[("hx", s), wk], writes=[ak])
                o = pp[ppi % 2]
                ok = ("pp", ppi % 2)
                oT = ppT[ppi % 2]
                oTk = ("ppT", ppi % 2)
                ppi += 1
                if c in V_CH or c in PLAIN_QK:
                    p.op("act", lambda e, o=o, a=a: e.activation(out=o[:], in_=a[:], func=AF.Copy), reads=[ak], writes=[ok])
                else:
                    isB = c in ROPE_B
                    r = rt[ppi % 2]
                    rk = ("rt", ppi % 2)
                    p.dma("sp", r[:], ropet[t], writes=[rk])
                    src = a
                    srck = ak
                    if isB:
                        gi = 0 if c < 9 else 1
                        for h in range(4):
                            p.op("act", lambda e, h=h, a=a: e.activation(
                                out=t1[:, h * 128:(h + 1) * 128], in_=a[:, h * 128:(h + 1) * 128], func=AF.Square, accum_out=ss4[:, h:h + 1]),
                                reads=[ak], writes=["t1", "ss4"])
                        p.op("dve", lambda e: e.tensor_scalar(out=ss4[:], in0=ss4[:], scalar1=1.0 / 128, scalar2=EPS, op0=ALU.mult, op1=ALU.add),
                             reads=["ss4"], writes=["ss4"])
                        p.op("act", lambda e: e.activation(out=ss4[:], in_=ss4[:], func=AF.Sqrt), reads=["ss4"], writes=["ss4"])
                        p.op("dve", lambda e: e.reciprocal(out=ss4[:], in_=ss4[:]), reads=["ss4"], writes=["ss4"])
                        for h in range(4):
                            p.op("dve", lambda e, h=h, gi=gi, a=a: e.scalar_tensor_tensor(
                                out=nrm[:, h * 128:(h + 1) * 128], in0=a[:, h * 128:(h + 1) * 128], scalar=ss4[:, h:h + 1],
                                in1=bn[:, gi, :], op0=ALU.mult, op1=ALU.mult),
                                reads=[ak, "ss4", "bn"], writes=["nrm"])
                        src = nrm
                        srck = "nrm"
                    ci, si = (2, 3) if isB else (0, 1)
                    hw = 32 if isB else 16
                    nb = 128 // (2 * hw)
                    Cb = r[:, ci, :].unsqueeze(1).broadcast_to([128, 4, 128])
                    sv = src[:].rearrange("p (h b two w) -> p h b two w", h=4, b=nb, two=2, w=hw)
                    Sv = r[:, si, :].rearrange("p (b two w) -> p b two w", b=nb, two=2, w=hw)
                    t2v = t2[:].rearrange("p (h b two w) -> p h b two w", h=4, b=nb, two=2, w=hw)
                    p.op("dve", lambda e, src=src, Cb=Cb: e.tensor_tensor(out=t1[:].rearrange("p (h d) -> p h d", h=4), in0=src[:].rearrange("p (h d) -> p h d", h=4), in1=Cb, op=ALU.mult),
                         reads=[srck, rk], writes=["t1"])
                    for h in range(4):
                        for half in range(2):
                            p.op("dve", lambda e, h=h, half=half, sv=sv, Sv=Sv, t2v=t2v: e.tensor_tensor(
                                out=t2v[:, h, :, half, :], in0=sv[:, h, :, 1 - half, :], in1=Sv[:, :, half, :], op=ALU.mult),
                                reads=[srck, rk], writes=["t2"])
                    p.op("dve", lambda e, o=o: e.tensor_tensor(out=o[:], in0=t1[:], in1=t2[:], op=ALU.add),
                         reads=["t1", "t2"], writes=[ok])
                if c in V_CH:
                    p.dma("sp", vtok[t * 128:(t + 1) * 128, VSLOT[c]:VSLOT[c] + 512], o[:], reads=[ok], writes=[("vtok", t, c)],
                          is_output=(out_kind == "ExternalOutput"), store=True)
                else:
                    tpt = tp[ppi % 2]
                    tpk = ("tp", ppi % 2)
                    for h in range(4):
                        p.op("pe", lambda e, h=h, o=o, tpt=tpt: e.transpose(out=tpt[:, h, :], in_=o[:, h * 128:(h + 1) * 128], identity=ident[:]),
                             reads=[ok, "ident"], writes=[tpk])
                    p.op("act", lambda e, oT=oT, tpt=tpt: e.activation(out=oT[:], in_=tpt[:], func=AF.Copy), reads=[tpk], writes=[oTk])
                    p.dma("sp", qkT[QSLOT[c]:QSLOT[c] + 4, :, t * 128:(t + 1) * 128].rearrange("h d t -> d h t"), oT[:], reads=[oTk],
                          writes=[("qkT", t, c)], is_output=(out_kind == "ExternalOutput"), store=True)
    return p


def rope_tables():
    n = np.arange(SEQ)
    row = (n // 64).astype(np.float64)
    col = (n % 64).astype(np.float64)
    out = np.zeros((NT, 128, 4, 128), np.float32)
    out[0:2, :, 0, :] = 1.0
    out[0:2, :, 2, :] = 1.0

    def blk(pos, half):
        fr = 10000.0 ** (-np.arange(half, dtype=np.float32) / half)
        ang = pos.astype(np.float32)[:, None] * fr[None, :]
        c, s_ = np.cos(ang), np.sin(ang)
        return np.concatenate([c, c], 1), np.concatenate([-s_, s_], 1)

    cr, sr = blk(row, 16)
    cc, sc = blk(col, 16)
    CA = np.concatenate([cr, cc, cr, cc], 1)
    SA = np.concatenate([sr, sc, sr, sc], 1)
    cr, sr = blk(row, 32)
    cc, sc = blk(col, 32)
    CB = np.concatenate([cr, cc], 1)
    SB = np.concatenate([sr, sc], 1)
    lat = np.stack([CA, SA, CB, SB], 1).reshape(32, 128, 4, 128)
    out[2:] = lat
    return out


def fm(v):
    return np.ascontiguousarray(np.asarray(v, np.float32).reshape(32, 128).T)


NEG = -30000.0


LAY_FULL = {
    "A": [(h, 8 + h, 128 * h, 128 * h) for h in range(8)],
    "B": [(16 + h, 28 + h // 3, 1024 + 128 * (h // 3), 1024 + 128 * h) for h in range(12)],
    "C": [(32 + h, 44 + h, 1536 + 128 * h, 2560 + 128 * h, h) for h in range(12)],
    "nq": 56, "vw": 3072, "ow": D, "ncb": 12,
}
LAY_Q4 = {
    "A": [(j, 2 + j, 128 * j, 128 * j) for j in range(2)],
    "B": [(4 + j, 7, 256, 256 + 128 * j) for j in range(3)],
    "C": [(8 + j, 11 + j, 384 + 128 * j, 640 + 128 * j, j) for j in range(3)],
    "nq": 14, "vw": 768, "ow": 1024, "ncb": 3,
}


def build_attn(heads=None, qblocks=None, crows=None, in_kind="ExternalInput", out_kind="ExternalOutput", lay=None):
    lay = lay or LAY_FULL
    p = Prog()
    qkT = p.dram("qkT", [lay["nq"], 128, NTOK], BF16, in_kind)
    vtok = p.dram("vtok", [NTOK, lay["vw"]], BF16, in_kind)
    alam = p.dram("alam", [128, 4, 64], F32, "ExternalInput")
    lconst = p.dram("lconst", [128, 2], F32, "ExternalInput")
    sublg = p.dram("sublg", [128, 128], F32, "ExternalInput")
    cb2d = p.dram("cb2d", [lay["ncb"], 128, 14, 64], F32, "ExternalInput")
    otok = p.dram("otok", [NTOK, lay["ow"]], BF16, out_kind)

    KT = p.sb("KT", [128, NTOK], BF16)
    QT = p.sb("QT", [128, NTOK], BF16)
    V1 = p.sb("V1", [128, NT, 129], BF16)
    V1o = p.sb("V1o", [128, 31, 129], BF16)
    cb2 = p.sb("cb2", [128, 14, 64], F32)
    al = p.sb("al", [128, 4, 64], F32)
    lc = p.sb("lc", [128, 2], F32)
    sg = p.sb("sg", [128, 128], F32)
    lam = p.sb("lam", [128, 4], F32)
    junk = p.sb("junk", [128, 128], F32)
    Pt = [p.sb(f"Pt{i}", [128, 512], BF16) for i in range(2)]
    sc = [p.sb(f"sc{i}", [128, 64], F32) for i in range(2)]
    o1 = p.sb("o1", [128, 4, 128], F32)
    o2 = p.sb("o2", [128, 4, 128], F32)
    rz = p.sb("rz", [128, 4], F32)
    ssq = p.sb("ssq", [128, 4], F32)
    ost = [p.sb(f"ost{i}", [128, 4, 128], BF16) for i in range(2)]
    pss = [p.ps(f"pss{i}", [128, 512], F32) for i in range(2)]
    acc = [p.ps(f"pacc{i}", [128, 512], F32) for i in range(4)]

    p.dma("sp", al[:], alam, writes=["al"])
    p.dma("sp", lc[:], lconst, writes=["lc"])
    p.dma("sp", sg[:], sublg, writes=["sg"])
    p.op("dve", lambda e: e.memset(V1[:, :, 128:129], 1.0), writes=["V1ones"])
    p.op("dve", lambda e: e.memset(V1o[:, :, 128:129], 1.0), writes=["V1oones"])
    for i in range(2):
        p.op("dve", lambda e, i=i: e.tensor_tensor(out=junk[:, 0:64], in0=al[:, 2 * i, :], in1=al[:, 2 * i + 1, :], op=ALU.mult),
             reads=["al"], writes=["junk"])
        p.op("dve", lambda e, i=i: e.reduce_sum(out=lam[:, i:i + 1], in_=junk[:, 0:64], axis=AX.X), reads=["junk"], writes=["lam"])
    p.op("act", lambda e: e.activation(out=lam[:, 0:2], in_=lam[:, 0:2], func=AF.Exp), reads=["lam"], writes=["lam"])
    p.op("dve", lambda e: e.tensor_tensor(out=lam[:, 2:3], in0=lam[:, 0:1], in1=lam[:, 1:2], op=ALU.subtract), reads=["lam"], writes=["lam"])
    p.op("dve", lambda e: e.tensor_tensor(out=lam[:, 2:3], in0=lam[:, 2:3], in1=lc[:, 0:1], op=ALU.add), reads=["lam", "lc"], writes=["lam"])
    p.op("dve", lambda e: e.tensor_scalar(out=lam[:, 3:4], in0=lam[:, 2:3], scalar1=-1.0, scalar2=None, op0=ALU.mult), reads=["lam"], writes=["lam"])
    p.op("dve", lambda e: e.tensor_scalar(out=sg[:], in0=sg[:], scalar1=lc[:, 1:2], scalar2=None, op0=ALU.mult), reads=["sg", "lc"], writes=["sg"])

    cnt = {"s": 0, "o": 0}

    def load_head(qh, kh, vcol, need_odd):
        p.dma("sp", QT[:], qkT[qh], writes=["QT"])
        p.dma("sp", KT[:], qkT[kh], writes=["KT"])
        p.dma("sp", V1[:, :, 0:128], vtok[:, vcol:vcol + 128].rearrange("(j p) d -> p j d", p=128), writes=["V1"])
        if need_odd:
            p.dma("sp", V1o[:, :, 0:128], vtok[320:320 + 31 * 128, vcol:vcol + 128].rearrange("(j p) d -> p j d", p=128), writes=["V1o"])

    def dense_pass(kr, q0, nq, ktiles, scale, fin):
        nqt = nq // 128
        for idx, j in enumerate(ktiles):
            s_ = pss[cnt["s"] % 2]
            sk = ("pss", cnt["s"] % 2)
            pt = Pt[cnt["s"] % 2]
            pk = ("Pt", cnt["s"] % 2)
            cnt["s"] += 1
            p.op("pe", lambda e, s_=s_, j=j: e.matmul(s_[:, 0:nq], lhsT=KT[kr[0]:kr[1], j * 128:(j + 1) * 128], rhs=QT[kr[0]:kr[1], q0:q0 + nq], start=True, stop=True),
                 reads=["KT", "QT"], writes=[sk])
            p.op("act", lambda e, s_=s_, pt=pt: e.activation(out=pt[:, 0:nq], in_=s_[:, 0:nq], func=AF.Exp, scale=scale), reads=[sk], writes=[pk])
            for qi in range(nqt):
                p.op("pe", lambda e, qi=qi, pt=pt, j=j, idx=idx: e.matmul(acc[qi][:, 0:129], lhsT=pt[:, qi * 128:(qi + 1) * 128], rhs=V1[:, j, :],
                                                                   start=(idx == 0), stop=(idx == len(ktiles) - 1)),
                     reads=[pk, "V1", "V1ones"], writes=[("acc", qi)])
        for qi in range(nqt):
            fin(qi)

    def fin_to(dst, dk):
        def f(qi):
            p.op("dve", lambda e, qi=qi: e.reciprocal(out=rz[:, qi:qi + 1], in_=acc[qi][:, 128:129]), reads=[("acc", qi)], writes=[("rz", qi)])
            p.op("act", lambda e, qi=qi: e.activation(out=dst[:, qi, :], in_=acc[qi][:, 0:128], func=AF.Copy, scale=rz[:, qi:qi + 1]),
                 reads=[("acc", qi), ("rz", qi)], writes=[(dk, qi)])
        return f

    def store(o, ok_, q0, nqt, col):
        p.dma("sp", otok[q0:q0 + nqt * 128, col:col + 128].rearrange("(j p) d -> p j d", p=128), o[:, 0:nqt, :],
              reads=[(ok_, qi) for qi in range(nqt)], writes=[("otok", q0, col)], is_output=(out_kind == "ExternalOutput"), store=True,
              stream=("d", ok_))

    blocks = [(0, 256, [0, 1])] + [(256 + 512 * i, 512, list(range(NT))) for i in range(8)]
    if qblocks is not None:
        blocks = [blocks[i] for i in qblocks]
    allheads = [(g_, h) for g_ in ("A", "B", "C") for h in range(len(lay[g_]))]
    if heads is not None:
        allheads = heads
    for grp, h in allheads:
        if grp == "A":
            load_head(lay["A"][h][0], lay["A"][h][1], lay["A"][h][2], False)
            col = lay["A"][h][3]
            for (q0, nq, kts) in blocks:
                nqt = nq // 128
                o = ost[cnt["o"] % 2]
                ok_ = ("ost", cnt["o"] % 2)
                cnt["o"] += 1
                dense_pass((0, 64), q0, nq, kts, 0.125, fin_to(o1, "o1"))
                dense_pass((64, 128), q0, nq, kts, 0.125, fin_to(o2, "o2"))
                for qi in range(nqt):
                    p.op("dve", lambda e, qi=qi: e.scalar_tensor_tensor(out=o1[:, qi, :], in0=o2[:, qi, :], scalar=lam[:, 3:4], in1=o1[:, qi, :], op0=ALU.mult, op1=ALU.add),
                         reads=[("o1", qi), ("o2", qi), "lam"], writes=[("o1", qi)])
                    p.op("act", lambda e, qi=qi: e.activation(out=junk[:], in_=o1[:, qi, :], func=AF.Square, accum_out=ssq[:, qi:qi + 1]),
                         reads=[("o1", qi)], writes=["junk", ("ssq", qi)])
                    p.op("dve", lambda e, qi=qi: e.tensor_scalar(out=ssq[:, qi:qi + 1], in0=ssq[:, qi:qi + 1], scalar1=1.0 / 128, scalar2=EPS, op0=ALU.mult, op1=ALU.add),
                         reads=[("ssq", qi)], writes=[("ssq", qi)])
                    p.op("act", lambda e, qi=qi: e.activation(out=ssq[:, qi:qi + 1], in_=ssq[:, qi:qi + 1], func=AF.Sqrt), reads=[("ssq", qi)], writes=[("ssq", qi)])
                    p.op("dve", lambda e, qi=qi: e.reciprocal(out=ssq[:, qi:qi + 1], in_=ssq[:, qi:qi + 1]), reads=[("ssq", qi)], writes=[("ssq", qi)])
                    p.op("dve", lambda e, qi=qi, o=o: e.scalar_tensor_tensor(out=o[:, qi, :], in0=o1[:, qi, :], scalar=ssq[:, qi:qi + 1], in1=sg[:], op0=ALU.mult, op1=ALU.mult),
                         reads=[("o1", qi), ("ssq", qi), "sg"], writes=[(ok_, qi)])
                store(o, ok_, q0, nqt, col)
        elif grp == "B":
            load_head(lay["B"][h][0], lay["B"][h][1], lay["B"][h][2], False)
            col = lay["B"][h][3]
            sc_ = 128 ** -0.5
            for (q0, nq, kts) in blocks:
                nqt = nq // 128
                o = ost[cnt["o"] % 2]
                ok_ = ("ost", cnt["o"] % 2)
                cnt["o"] += 1
                dense_pass((0, 128), q0, nq, kts, sc_, fin_to(o, ok_))
                store(o, ok_, q0, nqt, col)
        else:
            load_head(lay["C"][h][0], lay["C"][h][1], lay["C"][h][2], True)
            p.dma("sp", cb2[:], cb2d[lay["C"][h][4]], writes=["cb2"])
            col = lay["C"][h][3]
            sc_ = 128 ** -0.5
            if qblocks is None or 0 in qblocks:
                o = ost[cnt["o"] % 2]
                ok_ = ("ost", cnt["o"] % 2)
                cnt["o"] += 1
                dense_pass((0, 128), 0, 256, [0, 1], sc_, fin_to(o, ok_))
                store(o, ok_, 0, 2, col)
            rows = range(64) if crows is None else crows
            for r in rows:
                rs = min(max(r - 4, 0), 56)
                q0 = 256 + 64 * r
                half = r % 2
                if half == 0 or r == rows[0]:
                    o = ost[cnt["o"] % 2]
                    ok_ = ("ost", cnt["o"] % 2)
                    cnt["o"] += 1
                a = acc[r % 4]
                akey = ("acc", r % 4)
                kts = [("c", 0), ("c", 1)] + [("l", i) for i in range(4)]
                for idx, (kind, i) in enumerate(kts):
                    s_ = pss[cnt["s"] % 2]
                    sk = ("pss", cnt["s"] % 2)
                    pt = Pt[cnt["s"] % 2]
                    pk = ("Pt", cnt["s"] % 2)
                    scb = sc[cnt["s"] % 2]
                    sck = ("sc", cnt["s"] % 2)
                    cnt["s"] += 1
                    if kind == "c":
                        k0 = i * 128
                        vt = V1[:, i, :]
                        vk = "V1"
                    else:
                        row0 = rs + 2 * i
                        k0 = 256 + 64 * row0
                        if row0 % 2 == 0:
                            vt = V1[:, 2 + row0 // 2, :]
                            vk = "V1"
                        else:
                            vt = V1o[:, (row0 - 1) // 2, :]
                            vk = "V1o"
                    p.op("pe", lambda e, s_=s_, k0=k0, q0=q0: e.matmul(s_[:, 0:64], lhsT=KT[:, k0:k0 + 128], rhs=QT[:, q0:q0 + 64], start=True, stop=True),
                         reads=["KT", "QT"], writes=[sk])
                    if kind == "c":
                        p.op("act", lambda e, s_=s_, pt=pt: e.activation(out=pt[:, 0:64], in_=s_[:, 0:64], func=AF.Exp, scale=sc_), reads=[sk], writes=[pk])
                    else:
                        di = rs + 2 * i - r + 7
                        p.op("dve", lambda e, s_=s_, scb=scb, di=di: e.scalar_tensor_tensor(out=scb[:], in0=s_[:, 0:64], scalar=sc_, in1=cb2[:, di, :], op0=ALU.mult, op1=ALU.add),
                             reads=[sk, "cb2"], writes=[sck])
                        p.op("act", lambda e, scb=scb, pt=pt: e.activation(out=pt[:, 0:64], in_=scb[:], func=AF.Exp), reads=[sck], writes=[pk])
                    p.op("pe", lambda e, a=a, pt=pt, vt=vt, idx=idx: e.matmul(a[0:64, 0:129], lhsT=pt[:, 0:64], rhs=vt, start=(idx == 0), stop=(idx == 5)),
                         reads=[pk, vk, vk + "ones"], writes=[akey])
                p.op("dve", lambda e, a=a, r=r: e.reciprocal(out=rz[0:64, r % 4:r % 4 + 1], in_=a[0:64, 128:129]), reads=[akey], writes=[("rz", r % 4)])
                p.op("act", lambda e, a=a, r=r, o=o, half=half: e.activation(out=o[0:64, half * 2, :], in_=a[0:64, 0:128], func=AF.Copy, scale=rz[0:64, r % 4:r % 4 + 1]),
                     reads=[akey, ("rz", r % 4)], writes=[(ok_, half)])
                p.dma("sp", otok[q0:q0 + 64, col:col + 128], o[0:64, half * 2, :], reads=[(ok_, half)], writes=[("otok", q0, col)],
                      is_output=(out_kind == "ExternalOutput"), store=True, stream=("d", ok_, half))
    return p


def build_cb2(rpb):
    qc = np.arange(64)
    cs = np.clip(qc - 8, 0, 48)
    kc = np.arange(64)
    valid = (kc[:, None] >= cs[None, :]) & (kc[:, None] < cs[None, :] + 16)
    dc = np.clip(kc[:, None] - qc[None, :] + 15, 0, 30)
    out = np.full((12, 128, 14, 64), NEG, np.float32)
    for di in range(14):
        for kr in range(2):
            v = rpb[:, di + kr][:, dc]
            out[:, kr * 64:(kr + 1) * 64, di, :] = np.where(valid[None], v, NEG)
    return out


GROUPS4 = [[0, 1]] + [list(range(2 + 4 * i, 6 + 4 * i)) for i in range(8)]
GROUPS9 = [list(range(0, 9)), list(range(9, 18)), list(range(18, 26)), list(range(26, 34))]


def build_outproj(groups=GROUPS9, ntok=NTOK, ctx_tiles=(0, 1)):
    p = Prog()
    otok = p.dram("otok", [ntok, D], BF16, "ExternalInput")
    xin = p.dram("xin", [ntok, D], F32, "ExternalInput")
    w_out = p.dram("w_out", [D, D], F32, "ExternalInput")
    gbc = p.dram("gbc", [2, 128, D], F32, "ExternalInput")
    identd = p.dram("identd", [128, 128], F32, "ExternalInput")
    xmid = p.dram("xmid", [ntok, D], F32, "ExternalOutput")
    GMAX = max(len(g) for g in groups)
    ident = p.sb("ident", [128, 128], BF16)
    ot = p.sb("ot", [128, D], BF16)
    OT = p.sb("OT", [128, 32, GMAX * 128], BF16)
    wb = [p.sb(f"wb{i}", [128, 32, 512], BF16) for i in range(2)]
    gt = p.sb("gt", [128, 2, 512], F32)
    xc = [p.sb(f"xc{i}", [128, 512], F32) for i in range(2)]
    ob = [p.sb(f"ob{i}", [128, 512], F32) for i in range(2)]
    tp = [p.ps(f"tp{i}", [128, 4, 128], BF16) for i in range(2)]
    acc = [p.ps(f"acc{i}", [128, 512], F32) for i in range(2)]
    p.dma("pool", ident[:], identd, writes=["ident"])
    wi = 0
    oi = 0
    for g in groups:
        for s, t in enumerate(g):
            p.dma("sp", ot[:], otok[t * 128:(t + 1) * 128, :], writes=["ot"])
            for k4 in range(8):
                tpt = tp[k4 % 2]
                tpk = ("tp", k4 % 2)
                for j in range(4):
                    k = k4 * 4 + j
                    p.op("pe", lambda e, k=k, j=j, tpt=tpt: e.transpose(out=tpt[:, j, :], in_=ot[:, k * 128:(k + 1) * 128], identity=ident[:]),
                         reads=["ot", "ident"], writes=[tpk])
                p.op("act", lambda e, k4=k4, s=s, tpt=tpt: e.activation(out=OT[:, k4 * 4:(k4 + 1) * 4, s * 128:(s + 1) * 128], in_=tpt[:], func=AF.Copy),
                     reads=[tpk], writes=[("OT", s)])
        for n in range(8):
            w = wb[wi % 2]
            wk = ("wb", wi % 2)
            wi += 1
            for kq in range(4):
                p.dma("pool", w[:, kq * 8:(kq + 1) * 8, :],
                      w_out[kq * 1024:(kq + 1) * 1024, n * 512:(n + 1) * 512].rearrange("(k p) n -> p k n", p=128), writes=[wk])
            p.dma("sp", gt[:], gbc[:, :, n * 512:(n + 1) * 512].rearrange("a p n -> p a n"), writes=["gt"])
            for s, t in enumerate(g):
                a = acc[oi % 2]
                ak = ("acc", oi % 2)
                x_ = xc[oi % 2]
                xk = ("xc", oi % 2)
                o_ = ob[oi % 2]
                ok_ = ("ob", oi % 2)
                oi += 1
                gi = 1 if t in ctx_tiles else 0
                p.dma("sp", x_[:], xin[t * 128:(t + 1) * 128, n * 512:(n + 1) * 512], writes=[xk])
                for k in range(32):
                    p.op("pe", lambda e, a=a, w=w, k=k, s=s: e.matmul(a[:], lhsT=OT[:, k, s * 128:(s + 1) * 128], rhs=w[:, k, :], start=(k == 0), stop=(k == 31)),
                         reads=[("OT", s), wk], writes=[ak])
                p.op("dve", lambda e, a=a, o_=o_, gi=gi: e.tensor_tensor(out=o_[:], in0=a[:], in1=gt[:, gi, :], op=ALU.mult), reads=[ak, "gt"], writes=[ok_])
                p.op("dve", lambda e, o_=o_, x_=x_: e.tensor_tensor(out=o_[:], in0=o_[:], in1=x_[:], op=ALU.add), reads=[ok_, xk], writes=[ok_])
                p.dma("sp", xmid[t * 128:(t + 1) * 128, n * 512:(n + 1) * 512], o_[:], reads=[ok_], writes=[("xmid", t, n)], is_output=True, store=True)
    return p


def build_router(niter=30):
    p = Prog()
    xmid = p.dram("xmid", [NTOK, D], F32, "ExternalInput")
    modv = p.dram("modv", [128, 5, 32], F32, "ExternalInput")
    identd = p.dram("identd", [128, 128], F32, "ExternalInput")
    w_r = p.dram("w_r", [D, NEXP], F32, "ExternalInput")
    hx2T = p.dram("hx2T", [32, 128, NTOK], BF16, "ExternalOutput")
    gateT = p.dram("gateT", [NEXP, NTOK], F32, "ExternalOutput")

    ident = p.sb("ident", [128, 128], BF16)
    identf = p.sb("identf", [128, 128], F32)
    mv = p.sb("mv", [128, 5, 32], F32)
    ab = p.sb("ab", [128, 4, 32], F32)
    wr = p.sb("wr", [128, 32, NEXP], BF16)
    xt = p.sb("xt", [128, D], F32)
    xn = p.sb("xn", [128, D], BF16)
    ss = p.sb("ss", [128, 1], F32)
    rstd = p.sb("rstd", [128, 1], F32)
    hT = [p.sb(f"hT{i}", [128, 32, 128], BF16) for i in range(2)]
    aff = p.sb("aff", [128, NEXP], F32)
    mx = p.sb("mx", [128, 2], F32)
    affT = p.sb("affT", [NEXP, NTOK], F32)
    gT = p.sb("gT", [NEXP, NTOK], F32)
    st = p.sb("st", [NEXP, 8], F32)
    tp = [p.ps(f"tp{i}", [128, 4, 128], BF16) for i in range(2)]
    lg = p.ps("lg", [128, NEXP], F32)
    tpf = p.ps("tpf", [NEXP, 128], F32)

    p.dma("pool", ident[:], identd, writes=["ident"])
    p.dma("sp", identf[:], identd, writes=["identf"])
    p.dma("sp", mv[:], modv, writes=["mv"])
    p.dma("pool", wr[:], w_r.rearrange("(k p) n -> p k n", p=128), writes=["wr"])
    for i, (sc, sh) in enumerate(((1, 2), (3, 4))):
        p.op("dve", lambda e, i=i, sc=sc: e.scalar_tensor_tensor(out=ab[:, 2 * i, :], in0=mv[:, sc, :], scalar=1.0, in1=mv[:, 0, :], op0=ALU.add, op1=ALU.mult),
             reads=["mv"], writes=["lab" if i == 0 else "cab"])
        p.op("dve", lambda e, i=i, sh=sh: e.tensor_copy(out=ab[:, 2 * i + 1, :], in_=mv[:, sh, :]), reads=["mv"], writes=["lab" if i == 0 else "cab"])

    for t in range(NT):
        isctx = t < 2
        h = hT[t % 2]
        tag = "hT%d" % (t % 2)
        emit_norm_T(p, xmid, t, xt, xn, ss, rstd, ident, tp, h, 0,
                    ab[:, 2, :] if isctx else ab[:, 0, :], ab[:, 3, :] if isctx else ab[:, 1, :], tag, "cab" if isctx else "lab")
        p.dma("sp", hx2T[:, :, t * 128:(t + 1) * 128].rearrange("k p t -> p k t"), h[:], reads=[(tag, 0)], writes=[("hx2T", t)], is_output=True, store=True)
        for k in range(32):
            p.op("pe", lambda e, k=k, h=h: e.matmul(lg[:], lhsT=h[:, k, :], rhs=wr[:, k, :], start=(k == 0), stop=(k == 31)),
                 reads=[(tag, 0), "wr"], writes=["lg"])
        p.op("dve", lambda e: e.reduce_max(out=mx[:, 0:1], in_=lg[:], axis=AX.X), reads=["lg"], writes=["mx"])
        p.op("dve", lambda e: e.tensor_scalar(out=mx[:, 0:1], in0=mx[:, 0:1], scalar1=-1.0, scalar2=None, op0=ALU.mult), reads=["mx"], writes=["mx"])
        p.op("act", lambda e: e.activation(out=aff[:], in_=lg[:], func=AF.Exp, bias=mx[:, 0:1], scale=1.0, accum_out=mx[:, 1:2]), reads=["lg", "mx"], writes=["aff", "mx"])
        p.op("dve", lambda e: e.reciprocal(out=mx[:, 1:2], in_=mx[:, 1:2]), reads=["mx"], writes=["mx"])
        p.op("dve", lambda e: e.tensor_scalar(out=aff[:], in0=aff[:], scalar1=mx[:, 1:2], scalar2=None, op0=ALU.mult), reads=["aff", "mx"], writes=["aff"])
        p.op("pe", lambda e: e.transpose(out=tpf[:], in_=aff[:], identity=identf[:]), reads=["aff", "identf"], writes=["tpf"])
        p.op("act", lambda e, t=t: e.activation(out=affT[:, t * 128:(t + 1) * 128], in_=tpf[:], func=AF.Copy), reads=["tpf"], writes=["affT"])

    junk = xt
    for (c0, c1, kk) in ((0, 256, 32), (256, NTOK, 512)):
        n = c1 - c0
        p.op("dve", lambda e: e.memset(st[:, 0:1], 0.0), writes=["st"])
        p.op("dve", lambda e: e.memset(st[:, 1:2], 1.0), reads=["st"], writes=["st"])
        for it in range(niter):
            p.op("dve", lambda e: e.tensor_tensor(out=st[:, 2:3], in0=st[:, 0:1], in1=st[:, 1:2], op=ALU.add), reads=["st"], writes=["st"])
            p.op("dve", lambda e: e.tensor_scalar(out=st[:, 2:3], in0=st[:, 2:3], scalar1=0.5, scalar2=None, op0=ALU.mult), reads=["st"], writes=["st"])
            p.op("dve", lambda e, c0=c0, c1=c1, n=n: e.tensor_scalar(out=junk[0:NEXP, 0:n], in0=affT[:, c0:c1], scalar1=st[:, 2:3], scalar2=None, op0=ALU.is_ge),
                 reads=["affT", "st", "xt"], writes=["xt"])
            p.op("dve", lambda e, n=n: e.reduce_sum(out=st[:, 3:4], in_=junk[0:NEXP, 0:n], axis=AX.X), reads=["xt", "st"], writes=["st"])
            p.op("dve", lambda e, kk=kk: e.tensor_scalar(out=st[:, 4:5], in0=st[:, 3:4], scalar1=float(kk) - 0.5, scalar2=None, op0=ALU.is_ge), reads=["st"], writes=["st"])
            p.op("dve", lambda e: e.tensor_tensor(out=st[:, 5:6], in0=st[:, 2:3], in1=st[:, 0:1], op=ALU.subtract), reads=["st"], writes=["st"])
            p.op("dve", lambda e: e.scalar_tensor_tensor(out=st[:, 0:1], in0=st[:, 5:6], scalar=st[:, 4:5], in1=st[:, 0:1], op0=ALU.mult, op1=ALU.add), reads=["st"], writes=["st"])
            p.op("dve", lambda e: e.tensor_tensor(out=st[:, 5:6], in0=st[:, 1:2], in1=st[:, 2:3], op=ALU.subtract), reads=["st"], writes=["st"])
            p.op("dve", lambda e: e.scalar_tensor_tensor(out=st[:, 1:2], in0=st[:, 5:6], scalar=st[:, 4:5], in1=st[:, 2:3], op0=ALU.mult, op1=ALU.add), reads=["st"], writes=["st"])
        p.op("dve", lambda e, c0=c0, c1=c1: e.tensor_scalar(out=gT[:, c0:c1], in0=affT[:, c0:c1], scalar1=st[:, 0:1], scalar2=None, op0=ALU.is_ge),
             reads=["affT", "st"], writes=["gT"])
        p.op("dve", lambda e, c0=c0, c1=c1: e.tensor_tensor(out=gT[:, c0:c1], in0=gT[:, c0:c1], in1=affT[:, c0:c1], op=ALU.mult), reads=["gT", "affT"], writes=["gT"])
    p.dma("sp", gateT, gT[:], reads=["gT"], writes=["gateT"], is_output=True, store=True)
    return p


FFN_Q4_NTOK = 9 * 128
FFN_Q4_GROUPS = [[0, 1, 2, 3], [4, 5, 6, 7], [8]]


def build_ffn(groups=GROUPS4, nexp=NEXP, ntok=NTOK, ctx_tiles=(0, 1)):
    p = Prog()
    xmid = p.dram("xmid", [ntok, D], F32, "ExternalInput")
    hx2T = p.dram("hx2T", [32, 128, ntok], BF16, "ExternalInput")
    gateT = p.dram("gateT", [NEXP, ntok], F32, "ExternalInput")
    wg_d = p.dram("wg_d", [NEXP, D, FF], F32, "ExternalInput")
    wu_d = p.dram("wu_d", [NEXP, D, FF], F32, "ExternalInput")
    wd_d = p.dram("wd_d", [NEXP, FF, D], F32, "ExternalInput")
    gbc = p.dram("gbc", [2, 128, D], F32, "ExternalInput")
    seld = p.dram("seld", [NEXP, NEXP, 128], F32, "ExternalInput")
    xout = p.dram("xout", [ntok, D], F32, "ExternalOutput")

    hxg = p.sb("hxg", [128, 32, 512], BF16)
    hid = p.sb("hid", [128, 3 * nexp, 512], BF16)
    wg = p.sb("wg", [128, 32, FF], BF16)
    wu = p.sb("wu", [128, 32, FF], BF16)
    wd = p.sb("wd", [128, 3 * nexp, 256], BF16)
    sel = p.sb("sel", [NEXP, NEXP, 128], F32)
    gts = p.sb("gts", [NEXP, 512], F32)
    sg = [p.sb(f"sg{i}", [128, 512], F32) for i in range(2)]
    gt = p.sb("gt", [128, 2, 256], F32)
    xc = [p.sb(f"xc{i}", [128, 256], F32) for i in range(2)]
    ob = [p.sb(f"ob{i}", [128, 256], F32) for i in range(2)]
    gps = [p.ps(f"gps{i}", [128, 512], F32) for i in range(2)]
    ups = [p.ps(f"ups{i}", [128, 512], F32) for i in range(2)]
    bps = p.ps("bps", [128, 512], F32)
    yps = [p.ps(f"yps{i}", [128, 256], F32) for i in range(2)]

    p.dma("sp", sel[:], seld, writes=["sel"])
    ci = 0
    oi = 0
    for g in groups:
        gs = len(g) * 128
        t0 = g[0] * 128
        p.dma("sp", hxg[:, :, 0:gs], hx2T[:, :, t0:t0 + gs].rearrange("k p t -> p k t"), writes=["hxg"])
        p.dma("sp", gts[:, 0:gs], gateT[:, t0:t0 + gs], writes=["gts"])
        for ex in range(nexp):
            for kq in range(4):
                p.dma("pool", wg[:, kq * 8:(kq + 1) * 8, :], wg_d[ex, kq * 1024:(kq + 1) * 1024, :].rearrange("(k p) n -> p k n", p=128), writes=["wg"])
            for kq in range(4):
                p.dma("pool", wu[:, kq * 8:(kq + 1) * 8, :], wu_d[ex, kq * 1024:(kq + 1) * 1024, :].rearrange("(k p) n -> p k n", p=128), writes=["wu"])
            p.op("pe", lambda e, ex=ex, gs=gs: e.matmul(bps[:, 0:gs], lhsT=sel[:, ex, :], rhs=gts[:, 0:gs], start=True, stop=True),
                 reads=["sel", "gts"], writes=["bps"])
            for fc in range(3):
                gp = gps[ci % 2]
                gk = ("gps", ci % 2)
                up = ups[ci % 2]
                uk = ("ups", ci % 2)
                s_ = sg[ci % 2]
                sk = ("sg", ci % 2)
                ci += 1
                for k in range(32):
                    p.op("pe", lambda e, gp=gp, k=k, fc=fc, gs=gs: e.matmul(gp[:, 0:gs], lhsT=wg[:, k, fc * 128:(fc + 1) * 128], rhs=hxg[:, k, 0:gs], start=(k == 0), stop=(k == 31)),
                         reads=["wg", "hxg"], writes=[gk])
                for k in range(32):
                    p.op("pe", lambda e, up=up, k=k, fc=fc, gs=gs: e.matmul(up[:, 0:gs], lhsT=wu[:, k, fc * 128:(fc + 1) * 128], rhs=hxg[:, k, 0:gs], start=(k == 0), stop=(k == 31)),
                         reads=["wu", "hxg"], writes=[uk])
                p.op("act", lambda e, gp=gp, s_=s_, gs=gs: e.activation(out=s_[:, 0:gs], in_=gp[:, 0:gs], func=AF.Silu), reads=[gk], writes=[sk])
                p.op("dve", lambda e, up=up, s_=s_, gs=gs: e.tensor_tensor(out=s_[:, 0:gs], in0=s_[:, 0:gs], in1=up[:, 0:gs], op=ALU.mult), reads=[sk, uk], writes=[sk])
                p.op("dve", lambda e, s_=s_, ex=ex, fc=fc, gs=gs: e.tensor_tensor(out=hid[:, ex * 3 + fc, 0:gs], in0=s_[:, 0:gs], in1=bps[:, 0:gs], op=ALU.mult),
                     reads=[sk, "bps"], writes=["hid"])
        for n in range(16):
            for eq in range(4):
                e0 = eq * (nexp // 4)
                e1 = (eq + 1) * (nexp // 4)
                p.dma("pool", wd[:, e0 * 3:e1 * 3, :], wd_d[e0:e1, :, n * 256:(n + 1) * 256].rearrange("e (c p) n -> p (e c) n", p=128), writes=["wd"])
            p.dma("sp", gt[:], gbc[:, :, n * 256:(n + 1) * 256].rearrange("a p n -> p a n"), writes=["gt"])
            for s, t in enumerate(g):
                y = yps[oi % 2]
                yk = ("yps", oi % 2)
                x_ = xc[oi % 2]
                xk = ("xc", oi % 2)
                o_ = ob[oi % 2]
                ok_ = ("ob", oi % 2)
                oi += 1
                gi = 1 if t in ctx_tiles else 0
                p.dma("sp", x_[:], xmid[t * 128:(t + 1) * 128, n * 256:(n + 1) * 256], writes=[xk])
                for j in range(3 * nexp):
                    p.op("pe", lambda e, y=y, j=j, s=s: e.matmul(y[:], lhsT=hid[:, j, s * 128:(s + 1) * 128], rhs=wd[:, j, :], start=(j == 0), stop=(j == 3 * nexp - 1)),
                         reads=["hid", "wd"], writes=[yk])
                p.op("dve", lambda e, y=y, o_=o_, gi=gi: e.tensor_tensor(out=o_[:], in0=y[:], in1=gt[:, gi, :], op=ALU.mult), reads=[yk, "gt"], writes=[ok_])
                p.op("dve", lambda e, o_=o_, x_=x_: e.tensor_tensor(out=o_[:], in0=o_[:], in1=x_[:], op=ALU.add), reads=[ok_, xk], writes=[ok_])
                p.dma("sp", xout[t * 128:(t + 1) * 128, n * 256:(n + 1) * 256], o_[:], reads=[ok_], writes=[("xout", t, n)], is_output=True, store=True)
    return p


def build_final():
    p = Prog()
    xin = p.dram("xin", [SEQ, D], F32, "ExternalInput")
    gbc = p.dram("gbc", [128, D], F32, "ExternalInput")
    out = p.dram("out", [SEQ, D], F32, "ExternalOutput")
    g = p.sb("g", [128, D], F32)
    xt = [p.sb(f"xt{i}", [128, D], F32) for i in range(2)]
    sq = p.sb("sq", [128, D], BF16)
    ss = p.sb("ss", [128, 2], F32)
    p.dma("sp", g[:], gbc, writes=["g"])
    for t in range(SEQ // 128):
        x_ = xt[t % 2]
        xk = ("xt", t % 2)
        sk = ("ss", t % 2)
        c = t % 2
        p.dma("sp", x_[:], xin[t * 128:(t + 1) * 128, :], writes=[xk])
        p.op("act", lambda e, x_=x_, c=c: e.activation(out=sq[:], in_=x_[:], func=AF.Square, accum_out=ss[:, c:c + 1]), reads=[xk], writes=["sq", sk])
        p.op("dve", lambda e, c=c: e.tensor_scalar(out=ss[:, c:c + 1], in0=ss[:, c:c + 1], scalar1=1.0 / D, scalar2=EPS, op0=ALU.mult, op1=ALU.add), reads=[sk], writes=[sk])
        p.op("act", lambda e, c=c: e.activation(out=ss[:, c:c + 1], in_=ss[:, c:c + 1], func=AF.Sqrt), reads=[sk], writes=[sk])
        p.op("dve", lambda e, c=c: e.reciprocal(out=ss[:, c:c + 1], in_=ss[:, c:c + 1]), reads=[sk], writes=[sk])
        p.op("dve", lambda e, x_=x_, c=c: e.scalar_tensor_tensor(out=x_[:], in0=x_[:], scalar=ss[:, c:c + 1], in1=g[:], op0=ALU.mult, op1=ALU.mult),
             reads=[xk, sk, "g"], writes=[xk])
        p.dma("sp", out[t * 128:(t + 1) * 128, :], x_[:], reads=[xk], writes=[("out", t)], is_output=True, store=True)
    return p


def tok_split(arr, axis):
    arr = np.asarray(arr)
    out = []
    for g in range(4):
        shp = list(arr.shape)
        shp[axis] = FFN_Q4_NTOK
        piece = np.zeros(shp, arr.dtype)
        src = [slice(None)] * arr.ndim
        dst = [slice(None)] * arr.ndim
        src[axis] = slice(CTX + 1024 * g, CTX + 1024 * (g + 1)); dst[axis] = slice(0, 1024)
        piece[tuple(dst)] = arr[tuple(src)]
        src[axis] = slice(64 * g, 64 * g + 64); dst[axis] = slice(1024, 1088)
        piece[tuple(dst)] = arr[tuple(src)]
        out.append(piece)
    return out


def tok_merge(parts, axis):
    shp = list(parts[0].shape)
    shp[axis] = NTOK
    full = np.empty(shp, parts[0].dtype)
    for g in range(4):
        src = [slice(None)] * full.ndim
        dst = [slice(None)] * full.ndim
        dst[axis] = slice(CTX + 1024 * g, CTX + 1024 * (g + 1)); src[axis] = slice(0, 1024)
        full[tuple(dst)] = parts[g][tuple(src)]
        dst[axis] = slice(64 * g, 64 * g + 64); src[axis] = slice(1024, 1088)
        full[tuple(dst)] = parts[g][tuple(src)]
    return full


def _run2(prog, in_maps):
    nc = prog.finish()
    res = run_bass_kernel_spmd(nc, in_maps, core_ids=[0, 1])
    return res.results


def _bc(v, n=128):
    v = np.asarray(v, np.float32)
    return np.ascontiguousarray(np.broadcast_to(v[None], (n,) + v.shape))


def kernel(x, c, ctx, c_ctx, w_ada, b_ada, norm1_g, norm2_g, w_in, w_out, a_lambda, a_subln_g,
           b_q_norm_g, b_k_norm_g, c_rpb, w_router, w_e_gate, w_e_up, w_e_down, final_g):
    f32 = lambda a: np.asarray(a, np.float32)
    x, c, ctx, c_ctx = f32(x), f32(c), f32(ctx), f32(c_ctx)
    mod = run_ada(c, c_ctx, f32(w_ada), f32(b_ada))
    state = [np.ascontiguousarray(np.concatenate([ctx[b], x[b]], 0)) for b in range(2)]
    ident = np.eye(128, dtype=np.float32)
    rt = rope_tables()
    sel = np.zeros((NEXP, NEXP, 128), np.float32)
    for k in range(NEXP):
        sel[k, k, :] = 1.0
    for l in range(DEPTH):
        sh1, sc1, g1, sh2, sc2, g2 = np.split(mod[l], 6, axis=1)
        lam_init = 0.8 - 0.6 * float(np.exp(-0.3 * l))
        bng = _bc(np.stack([f32(b_q_norm_g[l]), f32(b_k_norm_g[l])]))
        wl_in = np.ascontiguousarray(f32(w_in[l]))
        ims = []
        for b in range(2):
            modv = np.ascontiguousarray(np.stack([fm(norm1_g[l]), fm(sc1[b]), fm(sh1[b]), fm(sc1[2]), fm(sh1[2])], 1))
            xs = tok_split(state[b], 0)
            for g in range(4):
                rtc = np.ascontiguousarray(np.concatenate([rt[2 + 8 * g:2 + 8 * g + 8], rt[0:1]], 0))
                ims.append({"xin": xs[g], "w_in": wl_in, "modv": modv, "identd": ident, "ropet": rtc, "bng": bng})
        r8 = run_bass_kernel_spmd(build_inproj([list(range(9))], ntok=FFN_Q4_NTOK, nt=9, ctx_tiles=(8,)).finish(), ims, core_ids=list(range(8))).results
        ra = [{"qkT": tok_merge([r8[4 * b + g]["qkT"] for g in range(4)], 2), "vtok": tok_merge([r8[4 * b + g]["vtok"] for g in range(4)], 0)} for b in range(2)]
        del r8, ims
        del wl_in
        alam = _bc(f32(a_lambda[l]))
        lconst = _bc(np.array([lam_init, 1.0 - lam_init], np.float32))
        sublg = _bc(f32(a_subln_g[l]))
        cb2 = build_cb2(f32(c_rpb[l]))
        ims = []
        for b in range(2):
            qk, vt = ra[b]["qkT"], ra[b]["vtok"]
            for g in range(4):
                qs = ([2 * g + j for j in range(2)] + [8 + 2 * g + j for j in range(2)] + [16 + 3 * g + j for j in range(3)] + [28 + g]
                      + [32 + 3 * g + j for j in range(3)] + [44 + 3 * g + j for j in range(3)])
                vc = [2 * g + j for j in range(2)] + [8 + g] + [12 + 3 * g + j for j in range(3)]
                vsel = np.concatenate([np.arange(128 * v, 128 * v + 128) for v in vc])
                ims.append({"qkT": np.ascontiguousarray(qk[qs]), "vtok": np.ascontiguousarray(vt[:, vsel]), "alam": alam, "lconst": lconst,
                            "sublg": sublg, "cb2d": np.ascontiguousarray(cb2[3 * g:3 * g + 3])})
        r8 = run_bass_kernel_spmd(build_attn(lay=LAY_Q4).finish(), ims, core_ids=list(range(8))).results
        rb = []
        for b in range(2):
            o = np.empty((NTOK, D), dtype=r8[0]["otok"].dtype)
            for g in range(4):
                oc = r8[4 * b + g]["otok"]
                for j in range(2):
                    o[:, 128 * (2 * g + j):128 * (2 * g + j) + 128] = oc[:, 128 * j:128 * j + 128]
                for j in range(3):
                    o[:, 1024 + 128 * (3 * g + j):1024 + 128 * (3 * g + j) + 128] = oc[:, 256 + 128 * j:256 + 128 * j + 128]
                    o[:, 2560 + 128 * (3 * g + j):2560 + 128 * (3 * g + j) + 128] = oc[:, 640 + 128 * j:640 + 128 * j + 128]
            rb.append({"otok": o})
        del ra
        wl_out = np.ascontiguousarray(f32(w_out[l]))
        ims = []
        for b in range(2):
            gb = np.ascontiguousarray(np.stack([_bc(g1[b]), _bc(g1[2])]))
            os_ = tok_split(rb[b]["otok"], 0)
            xs = tok_split(state[b], 0)
            for g in range(4):
                ims.append({"otok": os_[g], "xin": xs[g], "w_out": wl_out, "gbc": gb, "identd": ident})
        r8 = run_bass_kernel_spmd(build_outproj([list(range(9))], ntok=FFN_Q4_NTOK, ctx_tiles=(8,)).finish(), ims, core_ids=list(range(8))).results
        del rb, wl_out, ims
        xmid = [tok_merge([r8[4 * b + g]["xmid"] for g in range(4)], 0) for b in range(2)]
        del r8
        wr = np.ascontiguousarray(f32(w_router[l]))
        ims = []
        for b in range(2):
            modv = np.ascontiguousarray(np.stack([fm(norm2_g[l]), fm(sc2[b]), fm(sh2[b]), fm(sc2[2]), fm(sh2[2])], 1))
            ims.append({"xmid": xmid[b], "modv": modv, "identd": ident, "w_r": wr})
        rr = _run2(build_router(), ims)
        wg = np.ascontiguousarray(f32(w_e_gate[l]))
        wu = np.ascontiguousarray(f32(w_e_up[l]))
        wd = np.ascontiguousarray(f32(w_e_down[l]))
        ims = []
        for b in range(2):
            gb = np.ascontiguousarray(np.stack([_bc(g2[b]), _bc(g2[2])]))
            hx, gt_ = rr[b]["hx2T"], rr[b]["gateT"]
            for g in range(4):
                l0, c0 = CTX + 1024 * g, 64 * g
                xm = np.zeros((FFN_Q4_NTOK, D), np.float32)
                xm[:1024] = xmid[b][l0:l0 + 1024]
                xm[1024:1088] = xmid[b][c0:c0 + 64]
                hxc = np.zeros((32, 128, FFN_Q4_NTOK), hx.dtype)
                hxc[:, :, :1024] = hx[:, :, l0:l0 + 1024]
                hxc[:, :, 1024:1088] = hx[:, :, c0:c0 + 64]
                gtc = np.zeros((NEXP, FFN_Q4_NTOK), np.float32)
                gtc[:, :1024] = gt_[:, l0:l0 + 1024]
                gtc[:, 1024:1088] = gt_[:, c0:c0 + 64]
                ims.append({"xmid": xm, "hx2T": hxc, "gateT": gtc, "wg_d": wg, "wu_d": wu, "wd_d": wd, "gbc": gb, "seld": sel})
        r8 = run_bass_kernel_spmd(build_ffn(groups=FFN_Q4_GROUPS, ntok=FFN_Q4_NTOK, ctx_tiles=(8,)).finish(), ims, core_ids=list(range(8))).results
        del rr, wg, wu, wd, ims
        state = []
        for b in range(2):
            st_ = np.empty((NTOK, D), np.float32)
            for g in range(4):
                xo = r8[4 * b + g]["xout"]
                st_[CTX + 1024 * g:CTX + 1024 * (g + 1)] = xo[:1024]
                st_[64 * g:64 * g + 64] = xo[1024:1088]
            state.append(st_)
        del r8
    ims = [{"xin": np.ascontiguousarray(state[b][CTX:]), "gbc": _bc(f32(final_g))} for b in range(2)]
    ro = _run2(build_final(), ims)
    return np.stack([ro[b]["out"] for b in range(2)], 0).astype(np.float32)
```
